# Optimizing a Trainium2 kernel written in Bass

```python
import math
import jax, jax.numpy as jnp
from jax import lax
import numpy as np

D_MODEL = 1024
BATCH = 16
SEQ = 2048
DEPTH = 2

CTX_LEN = 256
GRID_W = 64

CONV_CH = 256
CONV_WIDTH = 31
DIFF_HEADS = 4
DIFF_DH = 32
FOURIER_GROUPS = 4
FOURIER_CH = 64
WIN_Q_HEADS = 4
WIN_KV_HEADS = 2
WIN_REP = WIN_Q_HEADS // WIN_KV_HEADS
WIN_DH = 64
WINDOW = 128
BLOCK = 128
ROPE_BASE = 10000.0

DIFF_QK = DIFF_HEADS * 2 * DIFF_DH
DIFF_V = DIFF_HEADS * 2 * DIFF_DH
FOURIER_W = FOURIER_GROUPS * FOURIER_CH
WIN_Q = WIN_Q_HEADS * WIN_DH
WIN_KV = WIN_KV_HEADS * WIN_DH
SPLIT_SIZES = (CONV_CH, CONV_CH, DIFF_QK, DIFF_QK, DIFF_V, FOURIER_W, WIN_Q, WIN_KV, WIN_KV)
D_IN = 2 * CONV_CH + 2 * DIFF_QK + DIFF_V + FOURIER_W + WIN_Q + 2 * WIN_KV
D_MIX = CONV_CH + 2 * DIFF_HEADS * DIFF_DH + FOURIER_W + WIN_Q

N_EXPERTS = 16
N_EXPERT_GROUPS = 4
EXPERTS_PER_GROUP = N_EXPERTS // N_EXPERT_GROUPS
TOP_K = 2
D_EXPERT = 512

ALPHA = (2 * DEPTH) ** 0.25
BETA = (8 * DEPTH) ** -0.25
LN_EPS = 1e-5

kernel_name = "hybrid_parallel_mixer_dit_block"


def layer_norm(x, g=None, b=None):
    xf = x.astype(jnp.float32)
    mu = jnp.mean(xf, -1, keepdims=True)
    var = jnp.mean(jnp.square(xf - mu), -1, keepdims=True)
    y = (xf - mu) * lax.rsqrt(var + LN_EPS)
    if g is not None:
        y = y * g.astype(jnp.float32) + b.astype(jnp.float32)
    return y.astype(x.dtype)


def rms_norm(x, g):
    xf = x.astype(jnp.float32)
    y = xf * lax.rsqrt(jnp.mean(xf * xf, -1, keepdims=True) + LN_EPS) * g.astype(jnp.float32)
    return y.astype(x.dtype)


def modulate(x, shift, scale):
    return layer_norm(x) * (1.0 + scale) + shift


def split_columns(u):
    idx = np.cumsum(SPLIT_SIZES)[:-1].tolist()
    return jnp.split(u, idx, axis=-1)


def axial_rope_tables(row_pos, col_pos, head_dim):
    n_axis = head_dim // 4
    inv = ROPE_BASE ** (-jnp.arange(n_axis, dtype=jnp.float32) / n_axis)
    ang = jnp.concatenate([row_pos[:, None] * inv, col_pos[:, None] * inv], -1)
    return jnp.cos(ang), jnp.sin(ang)


def apply_rope(x, cos, sin):
    xf = x.astype(jnp.float32)
    x1, x2 = xf[..., 0::2], xf[..., 1::2]
    y = jnp.stack([x1 * cos - x2 * sin, x1 * sin + x2 * cos], -1).reshape(x.shape)
    return y.astype(x.dtype)


def conv_module(u_val, u_gate, w_dw, b_dw, g_n, b_n):
    g = u_val * jax.nn.sigmoid(u_gate)
    y = lax.conv_general_dilated(
        g, w_dw[:, None, :].astype(g.dtype), window_strides=(1,),
        padding=[(CONV_WIDTH // 2, CONV_WIDTH // 2)],
        dimension_numbers=("NWC", "WIO", "NWC"), feature_group_count=CONV_CH)
    y = y + b_dw
    return jax.nn.silu(layer_norm(y, g_n, b_n))


def _diff_attend(q, k, v, lam):
    s = jnp.einsum("bqhcd,bkhcd->bhcqk", q, k).astype(jnp.float32) * (DIFF_DH ** -0.5)
    p = jax.nn.softmax(s, axis=-1)
    a = p[:, :, 0] - lam * p[:, :, 1]
    return jnp.einsum("bhqk,bkhe->bqhe", a.astype(v.dtype), v)


def diff_attention_latent(q, k, v, kc, vc, lam, cos, sin):
    B_, N = q.shape[0], q.shape[1]
    cs, sn = cos[:, None, None, :], sin[:, None, None, :]
    q = apply_rope(q, cs, sn)
    k = apply_rope(k, cs, sn)
    k_all = jnp.concatenate([kc, k], axis=1)
    v_all = jnp.concatenate([vc, v], axis=1)
    nb = N // BLOCK
    q_blocks = jnp.moveaxis(q.reshape(B_, nb, BLOCK, DIFF_HEADS, 2, DIFF_DH), 1, 0)
    o = lax.map(lambda qb: _diff_attend(qb, k_all, v_all, lam), q_blocks)
    return jnp.moveaxis(o, 0, 1).reshape(B_, N, DIFF_HEADS, 2 * DIFF_DH)


def diff_finish(o, lam_init, subln_g):
    o = rms_norm(o, subln_g) * (1.0 - lam_init)
    return o.reshape(o.shape[0], o.shape[1], DIFF_HEADS * 2 * DIFF_DH)


def fourier_mix(u):
    B_, N, _ = u.shape
    z = u.astype(jnp.float32).reshape(B_, N, FOURIER_GROUPS, FOURIER_CH)
    y = jnp.fft.fft2(z, axes=(1, 3), norm="ortho").real
    return y.reshape(B_, N, FOURIER_W).astype(u.dtype)


def window_attention_latent(q, k, v, kc, vc, sink, cos, sin):
    B_, N = q.shape[0], q.shape[1]
    C = kc.shape[1]
    nb = N // BLOCK
    cs, sn = cos[:, None, :], sin[:, None, :]
    q = apply_rope(q, cs, sn)
    k = apply_rope(k, cs, sn)
    qg = q.reshape(B_, nb, BLOCK, WIN_KV_HEADS, WIN_REP, WIN_DH)
    pad = ((0, 0), (BLOCK, BLOCK), (0, 0), (0, 0))
    kb = jnp.pad(k, pad).reshape(B_, nb + 2, BLOCK, WIN_KV_HEADS, WIN_DH)
    vb = jnp.pad(v, pad).reshape(B_, nb + 2, BLOCK, WIN_KV_HEADS, WIN_DH)
    kw = jnp.concatenate([kb[:, :-2], kb[:, 1:-1], kb[:, 2:]], axis=2)
    vw = jnp.concatenate([vb[:, :-2], vb[:, 1:-1], vb[:, 2:]], axis=2)
    qpos = jnp.arange(nb)[:, None] * BLOCK + jnp.arange(BLOCK)[None, :]
    kpos = (jnp.arange(nb)[:, None] - 1) * BLOCK + jnp.arange(3 * BLOCK)[None, :]
    rel = kpos[:, None, :] - qpos[:, :, None]
    valid = (jnp.abs(rel) <= WINDOW) & (kpos[:, None, :] >= 0) & (kpos[:, None, :] < N)
    scale = WIN_DH ** -0.5
    s_loc = jnp.einsum("bnqgrd,bnkgd->bngrqk", qg, kw).astype(jnp.float32) * scale
    s_loc = jnp.where(valid[None, :, None, None], s_loc, -jnp.inf)
    s_ctx = jnp.einsum("bnqgrd,bcgd->bngrqc", qg, kc).astype(jnp.float32) * scale
    s_sink = jnp.broadcast_to(
        sink.astype(jnp.float32).reshape(1, 1, WIN_KV_HEADS, WIN_REP, 1, 1), s_loc.shape[:-1] + (1,))
    p = jax.nn.softmax(jnp.concatenate([s_loc, s_ctx, s_sink], -1), axis=-1)
    L = 3 * BLOCK
    o = (jnp.einsum("bngrqk,bnkgd->bnqgrd", p[..., :L].astype(v.dtype), vw)
         + jnp.einsum("bngrqc,bcgd->bnqgrd", p[..., L:L + C].astype(v.dtype), vc))
    return o.reshape(B_, N, WIN_Q)


def window_attention_context(qc, kc, vc, sink):
    B_, C = qc.shape[0], qc.shape[1]
    qg = qc.reshape(B_, C, WIN_KV_HEADS, WIN_REP, WIN_DH)
    s = jnp.einsum("bqgrd,bkgd->bgrqk", qg, kc).astype(jnp.float32) * (WIN_DH ** -0.5)
    s_sink = jnp.broadcast_to(sink.astype(jnp.float32).reshape(WIN_KV_HEADS, WIN_REP, 1, 1), s.shape[:-1] + (1,))
    p = jax.nn.softmax(jnp.concatenate([s, s_sink], -1), axis=-1)
    o = jnp.einsum("bgrqk,bkgd->bqgrd", p[..., :C].astype(vc.dtype), vc)
    return o.reshape(B_, C, WIN_Q)


def token_mixing(h, hc, w_in, w_out, conv_w, conv_b, conv_ng, conv_nb, lam, lam_init, subln_g, sink,
                 rope_d, rope_w, need_ctx):
    B_, N, _ = h.shape
    C = hc.shape[1]
    av, ag, dq, dk, dv, fz, wq, wk, wv = split_columns(h @ w_in)
    avc, agc, dqc, dkc, dvc, fzc, wqc, wkc, wvc = split_columns(hc @ w_in)
    dq = dq.reshape(B_, N, DIFF_HEADS, 2, DIFF_DH)
    dk = dk.reshape(B_, N, DIFF_HEADS, 2, DIFF_DH)
    dv = dv.reshape(B_, N, DIFF_HEADS, 2 * DIFF_DH)
    dqc = dqc.reshape(B_, C, DIFF_HEADS, 2, DIFF_DH)
    dkc = dkc.reshape(B_, C, DIFF_HEADS, 2, DIFF_DH)
    dvc = dvc.reshape(B_, C, DIFF_HEADS, 2 * DIFF_DH)
    wq = wq.reshape(B_, N, WIN_Q_HEADS, WIN_DH)
    wk = wk.reshape(B_, N, WIN_KV_HEADS, WIN_DH)
    wv = wv.reshape(B_, N, WIN_KV_HEADS, WIN_DH)
    wkc = wkc.reshape(B_, C, WIN_KV_HEADS, WIN_DH)
    wvc = wvc.reshape(B_, C, WIN_KV_HEADS, WIN_DH)

    y_conv = conv_module(av, ag, conv_w, conv_b, conv_ng, conv_nb)
    y_diff = diff_finish(diff_attention_latent(dq, dk, dv, dkc, dvc, lam, rope_d[0], rope_d[1]), lam_init, subln_g)
    y_four = fourier_mix(fz)
    y_win = window_attention_latent(wq, wk, wv, wkc, wvc, sink, rope_w[0], rope_w[1])
    y = jnp.concatenate([y_conv, y_diff, y_four, y_win], axis=-1) @ w_out
    if not need_ctx:
        return y, None

    yc_conv = conv_module(avc, agc, conv_w, conv_b, conv_ng, conv_nb)
    yc_diff = diff_finish(_diff_attend(dqc, dkc, dvc, lam), lam_init, subln_g)
    yc_four = fourier_mix(fzc)
    yc_win = window_attention_context(wqc.reshape(B_, C, WIN_Q_HEADS, WIN_DH), wkc, wvc, sink)
    yc = jnp.concatenate([yc_conv, yc_diff, yc_four, yc_win], axis=-1) @ w_out
    return y, yc


def moe_ffn(h, router_w, router_bias, w_gate, w_up, w_down):
    shp = h.shape
    t = h.reshape(-1, shp[-1])
    scores = jax.nn.sigmoid((t @ router_w).astype(jnp.float32))
    sel = scores + router_bias.astype(jnp.float32)
    grp = sel.reshape(-1, N_EXPERT_GROUPS, EXPERTS_PER_GROUP)
    grp_score = jnp.sum(lax.top_k(grp, TOP_K)[0], -1)
    best = jnp.argmax(grp_score, -1)
    in_grp = jnp.arange(N_EXPERT_GROUPS)[None, :] == best[:, None]
    masked = jnp.where(jnp.repeat(in_grp, EXPERTS_PER_GROUP, axis=-1), sel, -jnp.inf)
    _, idx = lax.top_k(masked, TOP_K)
    w = jnp.take_along_axis(scores, idx, -1)
    w = w / jnp.sum(w, -1, keepdims=True)
    combine = jnp.sum(jax.nn.one_hot(idx, N_EXPERTS, dtype=jnp.float32) * w[..., None], axis=1)
    combine = combine.astype(t.dtype)
    out = jnp.zeros_like(t)
    for e in range(N_EXPERTS):
        he = jax.nn.silu(t @ w_gate[e]) * (t @ w_up[e])
        out = out + combine[:, e:e + 1] * (he @ w_down[e])
    return out.reshape(shp)


def setup_inputs(seed: int = 0) -> dict:
    key = jax.random.key(seed)
    ks = jax.random.split(key, 24)
    f32 = jnp.float32
    D, E, F = D_MODEL, N_EXPERTS, D_EXPERT

    def nrm(k, shape, scale):
        return jax.random.normal(k, shape, f32) * scale

    return {
        "x": nrm(ks[0], (BATCH, SEQ, D), 1.0),
        "c": nrm(ks[1], (BATCH, D), 1.0),
        "ctx": nrm(ks[2], (BATCH, CTX_LEN, D), 1.0),
        "c_ctx": nrm(ks[3], (D,), 1.0),
        "w_mod": nrm(ks[4], (DEPTH, D, 6 * D), D ** -0.5),
        "b_mod": nrm(ks[5], (DEPTH, 6 * D), 0.02),
        "w_in": nrm(ks[6], (DEPTH, D, D_IN), D ** -0.5),
        "w_out": nrm(ks[7], (DEPTH, D_MIX, D), D_MIX ** -0.5 * BETA),
        "conv_w": nrm(ks[8], (DEPTH, CONV_WIDTH, CONV_CH), CONV_WIDTH ** -0.5),
        "conv_b": nrm(ks[9], (DEPTH, CONV_CH), 0.02),
        "conv_norm_g": 1.0 + nrm(ks[10], (DEPTH, CONV_CH), 0.02),
        "conv_norm_b": nrm(ks[11], (DEPTH, CONV_CH), 0.02),
        "diff_lambda": nrm(ks[12], (DEPTH, 4, DIFF_DH), 0.1),
        "diff_subln_g": 1.0 + nrm(ks[13], (DEPTH, 2 * DIFF_DH), 0.02),
        "win_sink": nrm(ks[14], (DEPTH, WIN_Q_HEADS), 0.5),
        "ln_mix_g": 1.0 + nrm(ks[15], (DEPTH, D), 0.02),
        "ln_mix_b": nrm(ks[16], (DEPTH, D), 0.02),
        "ln_ffn_g": 1.0 + nrm(ks[17], (DEPTH, D), 0.02),
        "ln_ffn_b": nrm(ks[18], (DEPTH, D), 0.02),
        "router_w": nrm(ks[19], (D, E), D ** -0.5),
        "router_bias": nrm(ks[20], (E,), 0.01),
        "exp_w_gate": nrm(ks[21], (DEPTH, E, D, F), D ** -0.5),
        "exp_w_up": nrm(ks[22], (DEPTH, E, D, F), D ** -0.5),
        "exp_w_down": nrm(ks[23], (DEPTH, E, F, D), F ** -0.5 * BETA),
    }


def reference(x, c, ctx, c_ctx, w_mod, b_mod, w_in, w_out, conv_w, conv_b, conv_norm_g, conv_norm_b,
              diff_lambda, diff_subln_g, win_sink, ln_mix_g, ln_mix_b, ln_ffn_g, ln_ffn_b,
              router_w, router_bias, exp_w_gate, exp_w_up, exp_w_down):
    n_lat = x.shape[1]
    ROWS = n_lat // GRID_W
    row_pos = jnp.repeat(jnp.arange(ROWS, dtype=jnp.float32), GRID_W)
    col_pos = jnp.tile(jnp.arange(GRID_W, dtype=jnp.float32), ROWS)
    rope_d = axial_rope_tables(row_pos, col_pos, DIFF_DH)
    rope_w = axial_rope_tables(row_pos, col_pos, WIN_DH)

    xc = ctx
    for l in range(DEPTH):
        need_ctx = l < DEPTH - 1
        mod = jax.nn.silu(c) @ w_mod[l] + b_mod[l]
        mod_c = jax.nn.silu(c_ctx) @ w_mod[l] + b_mod[l]
        sh1, sc1, g1, sh2, sc2, g2 = jnp.split(mod[:, None, :], 6, axis=-1)
        csh1, csc1, cg1, csh2, csc2, cg2 = jnp.split(mod_c, 6, axis=-1)

        lam_init = 0.8 - 0.6 * math.exp(-0.3 * l)
        lv = diff_lambda[l].astype(jnp.float32)
        lam = jnp.exp(jnp.sum(lv[0] * lv[1])) - jnp.exp(jnp.sum(lv[2] * lv[3])) + lam_init

        h = modulate(x, sh1, sc1)
        hc = modulate(xc, csh1, csc1)
        y, yc = token_mixing(h, hc, w_in[l], w_out[l], conv_w[l], conv_b[l], conv_norm_g[l], conv_norm_b[l],
                             lam, lam_init, diff_subln_g[l], win_sink[l], rope_d, rope_w, need_ctx)
        x = layer_norm(ALPHA * x + g1 * y, ln_mix_g[l], ln_mix_b[l])
        h = modulate(x, sh2, sc2)
        x = layer_norm(ALPHA * x + g2 * moe_ffn(h, router_w, router_bias, exp_w_gate[l], exp_w_up[l], exp_w_down[l]),
                       ln_ffn_g[l], ln_ffn_b[l])
        if need_ctx:
            xc = layer_norm(ALPHA * xc + cg1 * yc, ln_mix_g[l], ln_mix_b[l])
            hc = modulate(xc, csh2, csc2)
            xc = layer_norm(ALPHA * xc + cg2 * moe_ffn(hc, router_w, router_bias, exp_w_gate[l], exp_w_up[l], exp_w_down[l]),
                            ln_ffn_g[l], ln_ffn_b[l])
    return x
```

```python
import math
import numpy as np
import ml_dtypes
import concourse.bass as bass
import concourse.mybir as mybir
from concourse.bass_utils import run_bass_kernel_spmd

F32 = mybir.dt.float32
BF16 = mybir.dt.bfloat16
AF = mybir.ActivationFunctionType
ALU = mybir.AluOpType
AX = mybir.AxisListType

D = 1024
SEQ = 2048
CTX = 256
T = SEQ + CTX
NT = T // 128
DEPTH = 2
NCORE = 8
BPC = 2
E = 16
FE = 512
ALPHA = (2 * DEPTH) ** 0.25
EPS = 1e-5
NCH = 29
BLKS = [(0, 256), (256, 512), (768, 512), (1280, 512), (1792, 512)]
BIG = 1.0e4


class Buf:
    def __init__(self, name, full):
        self.name = name
        self.full = full
        self.w = {}
        self.wf = {}
        self.r = {}

    def __getitem__(self, key):
        return V(self.full[key], self)

    @property
    def v(self):
        return V(self.full, self)


class V:
    def __init__(self, ap, buf):
        self.ap = ap
        self.buf = buf

    def __getitem__(self, key):
        return V(self.ap[key], self.buf)

    def bc(self, shape):
        return V(self.ap.to_broadcast(shape), self.buf)

    def re(self, s, **kw):
        return V(self.ap.rearrange(s, **kw), self.buf)


class Eng:
    def __init__(self, name, semkey):
        self.name = name
        self.semkey = semkey
        self.count = 0
        self.seen = {}
        self.ops = []


class Prog:
    NDMA = 40

    def __init__(self, nc):
        self.nc = nc
        self.sems = []
        self.E = {}
        for n in ["pe", "act", "dve", "pool", "sp"]:
            self.sems.append(nc.alloc_semaphore("sem_" + n))
            self.E[n] = Eng(n, len(self.sems) - 1)
        self.dma_sems = []
        for i in range(self.NDMA):
            self.sems.append(nc.alloc_semaphore("sem_dma%d" % i))
            self.dma_sems.append(len(self.sems) - 1)
        self.dma_val = {s: 0 for s in self.dma_sems}
        self.dma_next = 0
        self.n_ins = 0

    def _deps(self, eng, reads, writes, partial, extra=()):
        deps = {}

        def need(s, v):
            if v > deps.get(s, 0):
                deps[s] = v

        for b in reads:
            for s, v in b.w.items():
                need(s, v)
        for b in writes:
            if not partial:
                for s, v in b.w.items():
                    need(s, v)
            else:
                for s, v in b.wf.items():
                    need(s, v)
            for s, v in b.r.items():
                need(s, v)
        for s, v in extra:
            need(s, v)
        if eng.name == "pe":
            deps.pop(eng.semkey, None)
        waits = []
        for s, v in deps.items():
            if eng.seen.get(s, 0) < v:
                waits.append((s, v))
                eng.seen[s] = v
        return waits

    def _update(self, tok, reads, writes, partial):
        s, v = tok
        for b in reads:
            if b.r.get(s, 0) < v:
                b.r[s] = v
        for b in writes:
            if partial:
                if b.w.get(s, 0) < v:
                    b.w[s] = v
            else:
                b.w = {s: v}
                b.wf = {s: v}
                b.r = {}

    def emit(self, en, fn, reads, writes, partial=False):
        eng = self.E[en]
        waits = self._deps(eng, reads, writes, partial)
        eng.count += 1
        tok = (eng.semkey, eng.count)
        eng.ops.append((waits, fn, (eng.semkey, 1)))
        self._update(tok, reads, writes, partial)
        self.n_ins += 1

    def I(self, en, meth, _w=("out",), _partial=False, _rw=(), **kw):
        if en == "pool" and meth == "memset":
            self.n_pool_memset = getattr(self, "n_pool_memset", 0) + 1
        reads, writes, real = [], [], {}
        for k, v in kw.items():
            if isinstance(v, V):
                if k in _w or k == "accum_out":
                    writes.append(v.buf)
                else:
                    reads.append(v.buf)
                real[k] = v.ap
            else:
                real[k] = v

        def fn(e, meth=meth, real=real):
            return getattr(e, meth)(**real)

        self.emit(en, fn, reads, writes, _partial)

    def dma(self, out, in_, q="sp", partial=False, extra_reads=()):
        eng = self.E[q]
        s = self.dma_sems[self.dma_next]
        self.dma_next = (self.dma_next + 1) % self.NDMA
        prev = self.dma_val[s]
        extra = [(s, prev)] if prev > 0 else []
        rd = [in_.buf] + list(extra_reads)
        waits = self._deps(eng, rd, [out.buf], partial, extra)
        self.dma_val[s] = prev + 16
        tok = (s, prev + 16)
        oap, iap = out.ap, in_.ap

        def fn(e):
            return e.dma_start(out=oap, in_=iap)

        eng.ops.append((waits, fn, (s, 16)))
        self._update(tok, rd, [out.buf], partial)
        self.n_ins += 1

    def barrier(self, name=None):
        if not hasattr(self, "marks"):
            self.marks = []
        self.marks.append((name, {k: len(v.ops) for k, v in self.E.items()}))
        targets = [(e.semkey, e.count) for e in self.E.values() if e.count > 0]
        targets += [(s, v) for s, v in self.dma_val.items() if v > 0]
        for eng in self.E.values():
            waits = []
            for s, v in targets:
                if s == eng.semkey and eng.name == "pe":
                    continue
                if eng.seen.get(s, 0) < v:
                    waits.append((s, v))
                    eng.seen[s] = v
            if waits:
                eng.ops.append((waits, None, None))
        if getattr(self, "marker", None) is not None:
            self.I("pool", "memset", ap=self.marker.v, constant=float(len(self.marks)), _w=("ap",))
            self.marks[-1] = (name, dict(self.marks[-1][1], memset_idx=self.n_pool_memset))

    def replay(self, en, e):
        sems = self.sems
        for waits, fn, inc in self.E[en].ops:
            for s, v in waits:
                e.wait_ge(sems[s], v)
            if fn is not None:
                ins = fn(e)
                ins.then_inc(sems[inc[0]], inc[1])


class Arena:
    def __init__(self, nc, words):
        self.t = nc.alloc_sbuf_tensor("arena", [128, words], F32)
        self.words = words
        self.off = 0

    def mark(self):
        return self.off

    def reset(self, m):
        self.off = m

    def alloc(self, name, shape, dtype):
        n = 1
        for d in shape[1:]:
            n *= d
        nb = n * (2 if dtype == BF16 else 4)
        w = (nb + 3) // 4
        assert self.off + w <= self.words, ("arena overflow", name, self.off, w, self.words)
        ap = self.t[:, self.off:self.off + w]
        self.off += w
        if dtype == BF16:
            ap = ap.bitcast(BF16)[:, 0:n]
        elif dtype != F32:
            ap = ap.bitcast(dtype)[:, 0:n]
        if len(shape) == 3:
            ap = ap.rearrange("p (a b) -> p a b", a=shape[1])
        elif len(shape) == 4:
            ap = ap.rearrange("p (a b c) -> p a b c", a=shape[1], b=shape[2])
        ap = ap[0:shape[0]]
        b = Buf(name, ap)
        b.off = self.off - w
        b.words = w
        return b

    def span(self, bufs, j):
        b0 = bufs[0]
        for i, b in enumerate(bufs):
            assert b.off == b0.off + i * b0.words
        ap = self.t[:, b0.off:b0.off + len(bufs) * b0.words].rearrange("p (j d) -> p j d", j=j)
        return V(ap, b0)


def _perm_w_in():
    o_av, o_ag, o_dq, o_dk, o_dv, o_fz, o_wq, o_wk, o_wv = 0, 256, 512, 768, 1024, 1280, 1536, 1792, 1920
    sw = lambda a: (a ^ 1)
    cols = []
    cols += [o_av + i for i in range(256)]
    cols += [o_ag + i for i in range(256)]

    def dchunks(base, swap):
        out = []
        for ci in range(3):
            for slot in range(4):
                b = 3 * ci + slot
                if slot == 3 or b >= 8:
                    b = 0
                out += [base + b * 32 + (sw(d) if swap else d) for d in range(32)]
        return out

    cols += dchunks(o_dq, False)
    cols += dchunks(o_dq, True)
    cols += dchunks(o_dk, False)
    cols += dchunks(o_dk, True)
    cols += [o_dv + i for i in range(256)]
    cols += [o_fz + i for i in range(256)]
    cols += [o_wq + i for i in range(256)]
    cols += [o_wq + sw(i) for i in range(256)]
    kd = lambda g: [o_wk + g * 64 + (i % 64) for i in range(128)]
    kds = lambda g: [o_wk + g * 64 + sw(i % 64) for i in range(128)]
    cols += kd(0) + kd(1)
    cols += kds(0) + kds(1)
    cols += [o_wv + i for i in range(128)]
    return np.array(cols, dtype=np.int64)


def _rope_tables(dh, reps):
    n_axis = dh // 4
    inv = (10000.0 ** (-np.arange(n_axis, dtype=np.float32) / n_axis)).astype(np.float32)
    t = np.arange(SEQ)
    row = (t // 64).astype(np.float32)
    col = (t % 64).astype(np.float32)
    ang = np.concatenate([row[:, None] * inv[None, :], col[:, None] * inv[None, :]], -1)
    cos = np.cos(ang).astype(np.float32)
    sin = np.sin(ang).astype(np.float32)
    ct = np.zeros((dh, SEQ), np.float32)
    st = np.zeros((dh, SEQ), np.float32)
    for d in range(dh):
        ct[d] = cos[:, d // 2]
        st[d] = (-sin[:, d // 2]) if d % 2 == 0 else sin[:, d // 2]
    return np.tile(ct, (reps, 1)).copy(), np.tile(st, (reps, 1)).copy()


def _dft_tables(n):
    k = np.arange(n, dtype=np.int64)
    ph = (np.outer(k, k) % n).astype(np.float64) * (2.0 * np.pi / n)
    return np.cos(ph), np.sin(ph)


def _consts():
    cst = {}
    cd, sd = _rope_tables(32, 4)
    cw, sw = _rope_tables(64, 2)
    cst["rope"] = np.stack([cd, sd, cw, sw], 0).astype(np.float32)
    c64, s64 = _dft_tables(64)
    cs = np.zeros((128, 256), np.float64)
    for g in range(2):
        cs[g * 64:(g + 1) * 64, g * 64:(g + 1) * 64] = c64
        cs[g * 64:(g + 1) * 64, 128 + g * 64:128 + (g + 1) * 64] = s64
    cst["f64"] = cs.astype(ml_dtypes.bfloat16)
    cN, sN = _dft_tables(SEQ)
    def pack(m, nblk, bw, ntt):
        return m.reshape(ntt, 128, nblk, bw).transpose(2, 1, 0, 3)
    cst["fN"] = np.stack([pack(cN, 4, 512, 16), pack(-sN, 4, 512, 16)], 1).astype(ml_dtypes.bfloat16)
    cC, sC = _dft_tables(CTX)
    cst["fC"] = np.stack([pack(cC, 1, 256, 2)[0], pack(-sC, 1, 256, 2)[0]], 0).astype(ml_dtypes.bfloat16)
    j = np.arange(128)[:, None]
    i = np.arange(128)[None, :]
    m = np.stack([(j >= i), np.ones((128, 128), bool), (j <= i)], 1).astype(np.float32)
    cst["wmask"] = m.astype(ml_dtypes.bfloat16)
    cst["ident"] = np.eye(128, dtype=np.float32).astype(ml_dtypes.bfloat16)
    bm = np.zeros((128, 4), np.float32)
    for s_ in range(4):
        bm[32 * s_:32 * (s_ + 1), s_] = 1.0
    cst["bmask"] = bm
    return cst


def build_nc(debug=False, layers=(0, 1), bis=(0, 1), stop_after=None):
    nc = bass.Bass("TRN2", target_bir_lowering=False)

    def din(name, shape, dt=F32):
        return Buf(name, nc.dram_tensor(name, list(shape), dt, kind="ExternalInput").ap())

    def dscr(name, shape, dt=F32, kind="Internal"):
        return Buf(name, nc.dram_tensor(name, list(shape), dt, kind=kind).ap())

    x_in = din("x", [BPC, SEQ, D])
    ctx_in = din("ctx", [BPC, CTX, D])
    cT_in = din("cT", [128, 8, 3])
    w_mod = din("w_mod", [DEPTH, 128, 8, 6 * D])
    b_mod = din("b_mod", [DEPTH, 6 * D])
    w_inx = din("w_inx", [DEPTH, NCH, 128, 8, 128])
    w_out = din("w_out", [DEPTH, 128, 8, D])
    conv_w = din("conv_w", [DEPTH, 2, 128, 31])
    conv_v = din("conv_v", [DEPTH, 2, 128, 3])
    dlam = din("dlam", [DEPTH, 128])
    dsub = din("dsub", [DEPTH, 64])
    wsink = din("wsink", [DEPTH, 4])
    lnv = din("lnv", [DEPTH, 4, D])
    router_w = din("router_w", [128, 8, E])
    router_b = din("router_b", [E])
    wg = din("wg", [DEPTH, E, 128, 8, FE])
    wu = din("wu", [DEPTH, E, 128, 8, FE])
    wd = din("wd", [DEPTH, E, 128, 4, D])
    c_rope = din("c_rope", [4, 128, SEQ])
    c_f64 = din("c_f64", [128, 256], BF16)
    c_fN = din("c_fN", [4, 2, 128, 16, 512], BF16)
    c_fC = din("c_fC", [2, 128, 2, 256], BF16)
    c_wmask = din("c_wmask", [128, 3, 128], BF16)
    c_ident = din("c_ident", [128, 128], BF16)
    c_bmask = din("c_bmask", [128, 4])

    out_d = Buf("out", nc.dram_tensor("out", [BPC, SEQ, D], F32, kind="ExternalOutput").ap())
    dk = "ExternalOutput" if debug else "Internal"
    mod_d = dscr("mod_d", [DEPTH, 3, 6 * D], kind=dk)
    xres_d = [dscr("xres_d%d" % b, [T, D], kind=dk) for b in range(BPC)]
    x1_d = [dscr("x1_d%d" % b, [T, D], kind=dk) for b in range(BPC)]
    dbg = {}
    if debug:
        dbg["hT"] = dscr("dbg_hT", [128, 8, T], BF16, kind=dk)
        dbg["ymixT"] = dscr("dbg_ymixT", [128, 8, T], BF16, kind=dk)
        dbg["h2T"] = dscr("dbg_h2T", [128, 8, T], BF16, kind=dk)
        dbg["comb"] = dscr("dbg_comb", [128, NT, E], kind=dk)
        dbg["acc"] = dscr("dbg_acc", [T, D], kind=dk)

    P = Prog(nc)
    A = Arena(nc, 53000)
    I = P.I

    psS_ap = nc.alloc_psum_tensor("psS", [128, 2048], F32)[:]
    ps = [Buf("ps%d" % i, psS_ap[:, i * 512:(i + 1) * 512]) for i in range(4)]
    ps += [Buf("ps%d" % i, nc.alloc_psum_tensor("ps%d" % i, [128, 512], F32)[:]) for i in (4, 5)]
    psS2 = [Buf("psS2_%d" % k, psS_ap[:, k * 1024:(k + 1) * 1024].rearrange("p (a b) -> p a b", a=2)) for k in range(2)]
    pst = [Buf("pst%d" % i, nc.alloc_psum_tensor("pst%d" % i, [128, 512], F32)[:]) for i in range(2)]

    def bfv(b):
        return V(b.full.bitcast(BF16), b)
    ps_rr = [0]

    def nps(group=None):
        if group is None:
            i = ps_rr[0] % 6
            ps_rr[0] += 1
            return ps[i]
        return ps[group]

    pst_rr = [0]

    def npst():
        i = pst_rr[0] % 2
        pst_rr[0] += 1
        return pst[i]

    ident = A.alloc("ident", [128, 128], BF16)
    ones_f = A.alloc("ones_f", [128, 128], F32)
    hT = [A.alloc("hT%d" % i, [128, 8, n], BF16) for i, (t0, n) in enumerate(BLKS)]
    P.dma(ident.v, c_ident.v)
    ident_f = A.alloc("ident_f", [128, 128], F32)
    I("dve", "tensor_copy", out=ident_f.v, in_=ident.v)
    I("pool", "memset", ap=ones_f.v, constant=1.0, _w=("ap",))
    P.marker = A.alloc("marker", [128, 2], F32) if debug else None
    m_persist = A.mark()

    def blk_of(tt):
        t = tt * 128
        for i, (t0, n) in enumerate(BLKS):
            if t0 <= t < t0 + n:
                return i, t - t0
        raise ValueError

    def bcast_load(dst, src_ap_1d, n):
        P.dma(dst, V(src_ap_1d.ap.partition_broadcast(128), src_ap_1d.buf))

    def phase_mod():
        m0 = A.mark()
        cT = A.alloc("cT", [128, 8, 3], F32)
        sT = A.alloc("sT", [128, 8, 3], F32)
        wst = [A.alloc("wmst%d" % i, [128, 8, 512], F32) for i in range(2)]
        bsb = A.alloc("bsb", [3, 6 * D], F32)
        msb = A.alloc("msb", [3, 6 * D], F32)
        P.dma(cT.v, cT_in.v)
        I("act", "activation", out=sT.v, in_=cT.v, func=AF.Silu)
        for l in layers:
            for r in range(3):
                P.dma(bsb[r:r + 1, :], b_mod[l:l + 1, :], partial=True)
            for cb in range(12):
                w = wst[cb % 2]
                P.dma(w.v, w_mod[l, :, :, cb * 512:(cb + 1) * 512])
                pp = nps()
                for kc in range(8):
                    I("pe", "matmul", out=pp[0:3, :], lhsT=sT[:, kc, :], rhs=w[:, kc, :],
                      start=(kc == 0), stop=(kc == 7))
                I("dve", "tensor_tensor", out=msb[:, cb * 512:(cb + 1) * 512], in0=pp[0:3, :],
                  in1=bsb[:, cb * 512:(cb + 1) * 512], op=ALU.add, _partial=True)
            P.dma(mod_d[l], msb.v)
        P.barrier()
        A.reset(m0)

    def ln_stats(xt, st, mv, rstd, nmr):
        I("dve", "bn_stats", out=st[:, 0:6], in_=xt[:, 0:512])
        I("dve", "bn_stats", out=st[:, 6:12], in_=xt[:, 512:1024], _partial=True)
        I("dve", "bn_aggr", out=mv.v, in_=st.v)
        I("dve", "tensor_scalar_add", out=rstd.v, in0=mv[:, 1:2], scalar1=EPS)
        I("act", "activation", out=rstd.v, in_=rstd.v, func=AF.Sqrt)
        I("dve", "reciprocal", out=rstd.v, in_=rstd.v)
        I("dve", "scalar_tensor_tensor", out=nmr.v, in0=mv[:, 0:1], scalar=-1.0, in1=rstd.v,
          op0=ALU.mult, op1=ALU.mult)

    def batch_rstd(mvall, rs, nm, n):
        I("dve", "tensor_scalar_add", out=rs[:, 0:n], in0=mvall[:, 0:n, 1], scalar1=EPS)
        I("act", "activation", out=rs[:, 0:n], in_=rs[:, 0:n], func=AF.Sqrt)
        I("dve", "reciprocal", out=rs[:, 0:n], in_=rs[:, 0:n])
        I("dve", "scalar_tensor_tensor", out=nm[:, 0:n], in0=mvall[:, 0:n, 0], scalar=-1.0, in1=rs[:, 0:n],
          op0=ALU.mult, op1=ALU.mult)

    def act_sum(xt, junk, s1, i, sq=False):
        xv = xt if isinstance(xt, V) else xt.v
        I("act", "activation", out=junk.v, in_=xv, func=(AF.Square if sq else AF.Identity),
          accum_out=s1[:, i:i + 1])

    def batch_rstd2(s1, s2, mean, rs, n):
        I("dve", "tensor_scalar", out=mean[:, 0:n], in0=s1[:, 0:n], scalar1=1.0 / D, scalar2=None, op0=ALU.mult)
        I("dve", "tensor_tensor", out=rs[:, 0:n], in0=mean[:, 0:n], in1=mean[:, 0:n], op=ALU.mult)
        I("dve", "scalar_tensor_tensor", out=rs[:, 0:n], in0=s2[:, 0:n], scalar=1.0 / D, in1=rs[:, 0:n],
          op0=ALU.mult, op1=ALU.subtract)
        I("dve", "tensor_scalar_add", out=rs[:, 0:n], in0=rs[:, 0:n], scalar1=EPS)
        I("act", "activation", out=rs[:, 0:n], in_=rs[:, 0:n], func=AF.Sqrt)
        I("dve", "reciprocal", out=rs[:, 0:n], in_=rs[:, 0:n])

    def tile_stats(xt, stall, mvall, i):
        I("dve", "bn_stats", out=stall[:, i, 0:6], in_=xt[:, 0:512], _partial=True)
        I("dve", "bn_stats", out=stall[:, i, 6:12], in_=xt[:, 512:1024], _partial=True)
        I("dve", "bn_aggr", out=mvall[:, i, :], in_=stall[:, i, :], _partial=True)

    def transpose_tile(src_bf, dstT_list, tt):
        pt = bfv(npst())
        for kc in range(8):
            I("pe", "transpose", out=pt[:, kc * 128:(kc + 1) * 128], in_=src_bf[:, kc * 128:(kc + 1) * 128],
              identity=ident.v, _partial=True)
        bi_, off = blk_of(tt)
        I("act", "activation", out=dstT_list[bi_][:, :, off:off + 128],
          in_=pt.re("p (a b) -> p a b", a=8), func=AF.Copy, _partial=True)

    def x_src(l, bi, tt):
        if l == 0:
            if tt < 2:
                return ctx_in[bi, tt * 128:(tt + 1) * 128, :]
            return x_in[bi, (tt - 2) * 128:(tt - 1) * 128, :]
        return xres_d[bi][tt * 128:(tt + 1) * 128, :]

    def phase_mix(l, bi):
        last = (l == DEPTH - 1)
        need_ctx = not last
        m0 = A.mark()
        sc1 = A.alloc("sc1", [128, D], F32)
        sh1 = A.alloc("sh1", [128, D], F32)
        sc1c = A.alloc("sc1c", [128, D], F32)
        sh1c = A.alloc("sh1c", [128, D], F32)
        ymT = [A.alloc("ymT%d" % i, [128, 8, n], BF16) for i, (t0, n) in enumerate(BLKS)]
        mW = A.mark()
        wst = [A.alloc("wst%d" % i, [128, 8, 128], F32) for i in range(2)]
        wbf = [A.alloc("wbf%d" % i, [128, 8, 128], BF16) for i in range(2)]
        order = [18, 19, 4, 7, 10, 13, 5, 8, 11, 14, 6, 9, 12, 15, 20, 22, 21, 23, 24, 26, 25, 27]
        st_ = {"ptr": 0, "loaded": {}}

        def _load(i):
            c = order[i]
            s = i % 2
            P.dma(wst[s].v, w_inx[l, c])
            I("pool", "tensor_copy", out=wbf[s].v, in_=wst[s].v)
            st_["loaded"][i] = wbf[s]

        _load(0)

        def wchunk(c):
            i = st_["ptr"]
            assert order[i] == c, (order[i], c)
            st_["ptr"] += 1
            if i + 1 < len(order):
                _load(i + 1)
            return st_["loaded"][i]

        def proj_fm(c, blks, consume):
            w = wchunk(c)
            for bidx in blks:
                t0, n = BLKS[bidx]
                pp = nps()
                for kc in range(8):
                    I("pe", "matmul", out=pp[:, 0:n], lhsT=w[:, kc, :], rhs=hT[bidx][:, kc, :],
                      start=(kc == 0), stop=(kc == 7))
                consume(pp[:, 0:n], bidx, t0, n)

        bcast_load(sh1.v, mod_d[l, bi, 0:D], D)
        bcast_load(sc1.v, mod_d[l, bi, D:2 * D], D)
        bcast_load(sh1c.v, mod_d[l, 2, 0:D], D)
        bcast_load(sc1c.v, mod_d[l, 2, D:2 * D], D)
        I("dve", "tensor_scalar_add", out=sc1.v, in0=sc1.v, scalar1=1.0)
        I("dve", "tensor_scalar_add", out=sc1c.v, in0=sc1c.v, scalar1=1.0)

        m1 = A.mark()
        groups = [(0, 2)] + [(2 + 4 * g_, 4) for g_ in range(4)]
        XG = [A.alloc("XG%d" % gi, [128, cnt, D], F32) for gi, (tg, cnt) in enumerate(groups)]

        def Xv(tt):
            for gi, (tg, cnt) in enumerate(groups):
                if tg <= tt < tg + cnt:
                    return XG[gi][:, tt - tg, :]

        def x_src_group(tg, cnt):
            if l == 0:
                src = ctx_in[bi, 0:256, :] if tg == 0 else x_in[bi, (tg - 2) * 128:(tg - 2 + cnt) * 128, :]
            else:
                src = xres_d[bi][tg * 128:(tg + cnt) * 128, :]
            return src.re("(j p) d -> p j d", p=128)

        hb = [A.alloc("hb%d" % i, [128, D], BF16) for i in range(2)]
        s1 = A.alloc("s1", [128, NT], F32)
        s2 = A.alloc("s2", [128, NT], F32)
        mean = A.alloc("mean", [128, NT], F32)
        rs = A.alloc("rs", [128, NT], F32)
        junk = A.alloc("junk", [128, D], BF16)
        for gi, (tg, cnt) in enumerate(groups):
            P.dma(XG[gi].v, x_src_group(tg, cnt))
        for tt in range(NT):
            act_sum(Xv(tt), junk, s1, tt)
        for tt in range(NT):
            act_sum(Xv(tt), junk, s2, tt, sq=True)
        batch_rstd2(s1, s2, mean, rs, NT)
        for tt in range(NT):
            s = tt % 2
            scv, shv = (sc1c, sh1c) if tt < 2 else (sc1, sh1)
            I("dve", "scalar_tensor_tensor", out=Xv(tt), in0=Xv(tt), scalar=mean[:, tt:tt + 1], in1=scv.v,
              op0=ALU.subtract, op1=ALU.mult, _partial=True)
            I("dve", "scalar_tensor_tensor", out=hb[s].v, in0=Xv(tt), scalar=rs[:, tt:tt + 1], in1=shv.v,
              op0=ALU.mult, op1=ALU.add)
            transpose_tile(hb[s], hT, tt)
        if debug and l == layers[0] and bi == bis[0]:
            for i, (t0, n) in enumerate(BLKS):
                P.dma(dbg["hT"][:, :, t0:t0 + n], hT[i].v, partial=True)
        P.barrier()
        A.reset(m1)
        if stop_after == "lnt":
            A.reset(m0)
            return

        segs = [("lat", [1, 2, 3, 4], 256, SEQ)] + ([("ctx", [0], 0, CTX)] if need_ctx else [])

        m1 = A.mark()
        cw = [A.alloc("cw%d" % c, [128, 31], F32) for c in range(2)]
        cv = [A.alloc("cv%d" % c, [128, 3], F32) for c in range(2)]
        for c in range(2):
            P.dma(cw[c].v, conv_w[l, c])
            P.dma(cv[c].v, conv_v[l, c])
        gb = [A.alloc("gb%d" % c, [128, SEQ + 30], BF16) for c in range(2)]
        dg = [A.alloc("dg%d" % c, [128, 31, 128], BF16) for c in range(2)]
        for c in range(2):
            for j in range(31):
                I("dve", "tensor_scalar", out=dg[c][:, j, :], in0=ident.v, scalar1=cw[c][:, j:j + 1],
                  scalar2=None, op0=ALU.mult, _partial=True)
        ya = [A.alloc("ya%d" % c, [128, SEQ], F32) for c in range(2)]
        sgb = [A.alloc("sgb%d" % i, [128, 512], F32) for i in range(2)]
        sq = A.alloc("sq", [128, 512], F32)
        mean_t = A.alloc("mean_t", [128, 512], F32)
        rstd_t = A.alloc("rstd_t", [128, 512], F32)
        tmp_t = A.alloc("tmp_t", [128, 512], F32)
        wkeep = [A.alloc("wkeep%d" % i, [128, 8, 128], BF16) for i in range(4)]
        wks = [A.alloc("wks%d" % i, [128, 8, 128], F32) for i in range(2)]
        for i, c in enumerate([0, 2, 1, 3]):
            P.dma(wks[i % 2].v, w_inx[l, c])
            I("act", "activation", out=wkeep[i].v, in_=wks[i % 2].v, func=AF.Copy)
        for (sname, blks, s0, sn) in segs:
            for c in range(2):
                I("pool", "memset", ap=gb[c][:, 0:15], constant=0.0, _w=("ap",), _partial=True)
                I("pool", "memset", ap=gb[c][:, 15 + sn:30 + sn], constant=0.0, _w=("ap",), _partial=True)
                for bidx in blks:
                    t0, n = BLKS[bidx]
                    pv = nps()
                    pg = nps()
                    for kc in range(8):
                        I("pe", "matmul", out=pv[:, 0:n], lhsT=wkeep[2 * c][:, kc, :], rhs=hT[bidx][:, kc, :],
                          start=(kc == 0), stop=(kc == 7))
                    for kc in range(8):
                        I("pe", "matmul", out=pg[:, 0:n], lhsT=wkeep[2 * c + 1][:, kc, :], rhs=hT[bidx][:, kc, :],
                          start=(kc == 0), stop=(kc == 7))
                    sg = sgb[bidx % 2]
                    I("act", "activation", out=sg[:, 0:n], in_=pg[:, 0:n], func=AF.Sigmoid)
                    o = 15 + t0 - s0
                    I("dve", "tensor_tensor", out=gb[c][:, o:o + n], in0=pv[:, 0:n], in1=sg[:, 0:n], op=ALU.mult,
                      _partial=True)
                for bidx in blks:
                    t0, n = BLKS[bidx]
                    o = t0 - s0
                    pc = nps()
                    for j in range(31):
                        I("pe", "matmul", out=pc[:, 0:n], lhsT=dg[c][:, j, :], rhs=gb[c][:, o + j:o + j + n],
                          start=(j == 0), stop=(j == 30))
                    I("act", "activation", out=ya[c][:, o:o + n], in_=pc[:, 0:n], func=AF.Identity, bias=cv[c][:, 0:1],
                      _partial=True)
            for bidx in blks:
                t0, n = BLKS[bidx]
                o = t0 - s0
                p1 = nps()
                p2 = nps()
                for c in range(2):
                    I("pe", "matmul", out=p1[:, 0:n], lhsT=ones_f.v, rhs=ya[c][:, o:o + n], start=(c == 0), stop=(c == 1))
                for c in range(2):
                    I("act", "activation", out=sq[:, 0:n], in_=ya[c][:, o:o + n], func=AF.Square)
                    I("pe", "matmul", out=p2[:, 0:n], lhsT=ones_f.v, rhs=sq[:, 0:n], start=(c == 0), stop=(c == 1))
                I("dve", "tensor_scalar", out=mean_t[:, 0:n], in0=p1[:, 0:n], scalar1=1.0 / 256, scalar2=None, op0=ALU.mult)
                I("dve", "tensor_tensor", out=tmp_t[:, 0:n], in0=mean_t[:, 0:n], in1=mean_t[:, 0:n], op=ALU.mult)
                I("dve", "scalar_tensor_tensor", out=tmp_t[:, 0:n], in0=p2[:, 0:n], scalar=1.0 / 256, in1=tmp_t[:, 0:n],
                  op0=ALU.mult, op1=ALU.subtract)
                I("dve", "tensor_scalar_add", out=rstd_t[:, 0:n], in0=tmp_t[:, 0:n], scalar1=EPS)
                I("act", "activation", out=rstd_t[:, 0:n], in_=rstd_t[:, 0:n], func=AF.Sqrt)
                I("dve", "reciprocal", out=rstd_t[:, 0:n], in_=rstd_t[:, 0:n])
                for c in range(2):
                    I("dve", "tensor_tensor", out=tmp_t[:, 0:n], in0=ya[c][:, o:o + n], in1=mean_t[:, 0:n], op=ALU.subtract)
                    I("dve", "tensor_tensor", out=tmp_t[:, 0:n], in0=tmp_t[:, 0:n], in1=rstd_t[:, 0:n], op=ALU.mult)
                    I("act", "activation", out=ymT[bidx][:, c, :], in_=tmp_t[:, 0:n], func=AF.Silu,
                      bias=cv[c][:, 2:3], scale=cv[c][:, 1:2], _partial=True)
        P.barrier()
        A.reset(m1)

        m1 = A.mark()
        f64 = A.alloc("f64", [128, 256], BF16)
        P.dma(f64.v, c_f64.v)
        zT = [A.alloc("zT%d" % c, [128, T], BF16) for c in range(2)]
        AB = A.alloc("AB", [128, NT, 2, 256], BF16)
        ftabs = [[A.alloc("ftab%d_%d" % (k, i), [128, 16, 512], BF16) for i in range(2)] for k in range(2)]
        all_blks = [1, 2, 3, 4] + ([0] if need_ctx else [])
        for c in range(2):
            def cons(pv, bidx, t0, n, c=c):
                I("act", "activation", out=zT[c][:, t0:t0 + n], in_=pv, func=AF.Copy, _partial=True)
            proj_fm(18 + c, all_blks, cons)
        tiles = list(range(2, NT)) + ([0, 1] if need_ctx else [])
        for tt in tiles:
            for c in range(2):
                pp = nps()
                I("pe", "matmul", out=pp[:, 0:256], lhsT=zT[c][:, tt * 128:(tt + 1) * 128], rhs=f64.v, start=True, stop=True)
                I("dve", "tensor_copy", out=AB[:, tt, c, :], in_=pp[:, 0:256], _partial=True)
        for (sname, blks, s0, sn) in segs:
            ntt = sn // 128
            tt0 = s0 // 128
            scale = 1.0 / math.sqrt(sn * 64.0)
            def load_tab(kb_):
                ft = ftabs[kb_ % 2]
                if sname == "lat":
                    for j in range(2):
                        P.dma(ft[j].v, c_fN[kb_, j])
                else:
                    for j in range(2):
                        P.dma(ft[j][:, 0:2, 0:256], c_fC[j])

            load_tab(0)
            for kb, bidx in enumerate(blks):
                t0, n = BLKS[bidx]
                if kb + 1 < len(blks):
                    load_tab(kb + 1)
                ftab = ftabs[kb % 2]
                for c in range(2):
                    pp = nps()
                    for ti in range(ntt):
                        for j in range(2):
                            I("pe", "matmul", out=pp[:, 0:n], lhsT=AB[:, tt0 + ti, c, j * 128:(j + 1) * 128],
                              rhs=ftab[j][:, ti, 0:n], start=(ti == 0 and j == 0), stop=(ti == ntt - 1 and j == 1))
                    I("act", "activation", out=ymT[bidx][:, 4 + c, :], in_=pp[:, 0:n], func=AF.Copy, scale=scale,
                      _partial=True)
        P.barrier()
        A.reset(m1)

        def rope_proj(c_raw, c_sw, dst, tab_c, tab_s, t1, t2):
            res = {}

            def cons_raw(pv, bidx, t0, n):
                if bidx == 0:
                    I("act", "activation", out=dst[:, t0:t0 + n], in_=pv, func=AF.Copy, _partial=True)
                else:
                    I("dve", "tensor_tensor", out=res[bidx][:, 0:n], in0=pv, in1=tab_c[:, t0 - 256:t0 - 256 + n], op=ALU.mult)

            def cons_sw(pv, bidx, t0, n):
                I("dve", "tensor_tensor", out=t2[:, 0:n], in0=pv, in1=tab_s[:, t0 - 256:t0 - 256 + n], op=ALU.mult)
                I("dve", "tensor_tensor", out=dst[:, t0:t0 + n], in0=res[bidx][:, 0:n], in1=t2[:, 0:n], op=ALU.add,
                  _partial=True)

            for bidx in [1, 2, 3, 4]:
                res[bidx] = t1[bidx - 1]
            proj_fm(c_raw, [0, 1, 2, 3, 4], cons_raw)
            proj_fm(c_sw, [1, 2, 3, 4], cons_sw)

        def load_tm_weights(c0, ncols, name):
            wt = A.alloc(name, [128, 8, ncols], BF16)
            for j in range(ncols // 128):
                s = j % 2
                P.dma(wst[s].v, w_inx[l, c0 + j])
                I("act", "activation", out=wt[:, :, j * 128:(j + 1) * 128], in_=wst[s].v, func=AF.Copy, _partial=True)
            return wt

        def yT_from_tm(ytm, chunk0):
            tl = list(range(2, NT)) + ([0, 1] if need_ctx else [])
            for tt in tl:
                pt = bfv(npst())
                for j in range(2):
                    I("pe", "transpose", out=pt[:, j * 128:(j + 1) * 128], in_=ytm[:, tt, j * 128:(j + 1) * 128],
                      identity=ident.v, _partial=True)
                bi_, off = blk_of(tt)
                I("act", "activation", out=ymT[bi_][:, chunk0:chunk0 + 2, off:off + 128],
                  in_=pt[:, 0:256].re("p (a b) -> p a b", a=2), func=AF.Copy, _partial=True)

        m1 = A.mark()
        lam_init = 0.8 - 0.6 * math.exp(-0.3 * l)
        qT = [A.alloc("qT%d" % j, [128, T], BF16) for j in range(3)]
        kT = [A.alloc("kT%d" % j, [128, T], BF16) for j in range(3)]
        vaug = A.alloc("vaug", [128, NT, 4, 128], BF16)
        ydf = A.alloc("ydf", [128, NT, 256], BF16)
        PT = [A.alloc("PT%d" % i, [128, 2, 512], BF16) for i in range(3)]
        osT = [A.alloc("osT%d" % i, [128, 512], F32) for i in range(2)]
        osb = [A.alloc("osb%d" % i, [128, 2, 4, 65], F32) for i in range(2)]
        fin = [A.alloc("fin%d" % i, [128, 4, 64], F32) for i in range(2)]
        fsm = [A.alloc("fsm%d" % i, [128, 16], F32) for i in range(2)]
        qz = [A.alloc("qz%d" % i, [128, 512], BF16) for i in range(2)]
        bmask = A.alloc("bmask", [128, 4], F32)
        P.dma(bmask.v, c_bmask.v)
        lv = A.alloc("lv", [128, 128], F32)
        lsm = A.alloc("lsm", [128, 8], F32)
        lpr = A.alloc("lpr", [128, 64], F32)
        sub_g = A.alloc("sub_g", [128, 64], F32)
        m_prep = A.mark()
        ropec = A.alloc("ropec", [128, SEQ], F32)
        ropes = A.alloc("ropes", [128, SEQ], F32)
        P.dma(ropec.v, c_rope[0])
        P.dma(ropes.v, c_rope[1])
        t1 = [A.alloc("rt1_%d" % i, [128, 512], F32) for i in range(4)]
        t2 = A.alloc("rt2", [128, 512], F32)
        for j in range(3):
            rope_proj(4 + j, 7 + j, qT[j], ropec, ropes, t1, t2)
            rope_proj(10 + j, 13 + j, kT[j], ropec, ropes, t1, t2)
        wdv = load_tm_weights(16, 256, "wdv")
        I("pool", "memset", ap=vaug.v, constant=1.0, _w=("ap",))
        for tt in range(NT):
            bidx, off = blk_of(tt)
            pp = nps()
            for kc in range(8):
                I("pe", "matmul", out=pp[:, 0:256], lhsT=hT[bidx][:, kc, off:off + 128], rhs=wdv[:, kc, :],
                  start=(kc == 0), stop=(kc == 7))
            I("dve", "tensor_copy", out=vaug[:, tt, :, 0:64], in_=pp[:, 0:256].re("p (h e) -> p h e", h=4), _partial=True)
        bcast_load(lv.v, dlam[l], 128)
        I("dve", "tensor_tensor", out=lpr[:, 0:32], in0=lv[:, 0:32], in1=lv[:, 32:64], op=ALU.mult)
        I("dve", "tensor_tensor", out=lpr[:, 32:64], in0=lv[:, 64:96], in1=lv[:, 96:128], op=ALU.mult, _partial=True)
        I("dve", "reduce_sum", out=lsm[:, 0:2], in_=lpr.v.re("p (a b) -> p a b", a=2), axis=AX.X)
        I("act", "activation", out=lsm[:, 2:4], in_=lsm[:, 0:2], func=AF.Exp, _partial=True)
        I("dve", "tensor_tensor", out=lsm[:, 4:5], in0=lsm[:, 3:4], in1=lsm[:, 2:3], op=ALU.subtract, _partial=True)
        I("dve", "tensor_scalar_add", out=lsm[:, 5:6], in0=lsm[:, 4:5], scalar1=-lam_init, _partial=True)
        neg_lam = lsm[:, 5:6]
        bcast_load(sub_g.v, dsub[l], 64)
        I("dve", "tensor_scalar", out=sub_g.v, in0=sub_g.v, scalar1=(1.0 - lam_init), scalar2=None, op0=ALU.mult)
        pt_rr = 0
        sc_d = 32 ** -0.5
        fcnt = 0
        P.barrier("diff_prep")
        A.reset(m_prep)
        OT = [ps[4], ps[5]]
        O = [pst[0], pst[1]]
        units = []
        for (sname, blks, s0, sn) in segs:
            ktiles = list(range(NT)) if sname == "lat" else [0, 1]
            pairs = [ktiles[i:i + 2] for i in range(0, len(ktiles), 2)]
            for h in range(4):
                for bidx in blks:
                    for c in range(2):
                        units.append((ktiles, pairs, h, bidx, c))
        cnt_ = {"s": 0, "qz": 0, "pt": 0, "f": 0}

        def prep_q(u):
            ktiles, pairs, h, bidx, c = u
            t0, n = BLKS[bidx]
            j = (2 * h + c) // 3
            slot = (2 * h + c) % 3
            qzb = qz[cnt_["qz"] % 2]
            cnt_["qz"] += 1
            I("dve", "tensor_scalar", out=qzb[:, 0:n], in0=qT[j][:, t0:t0 + n], scalar1=bmask[:, slot:slot + 1],
              scalar2=None, op0=ALU.mult)
            return qzb

        def emit_S(u, pair, qzb):
            ktiles, pairs, h, bidx, c = u
            t0, n = BLKS[bidx]
            j = (2 * h + c) // 3
            S = psS2[cnt_["s"] % 2]
            cnt_["s"] += 1
            for uu, kt in enumerate(pair):
                I("pe", "matmul", out=S[:, uu, 0:n], lhsT=kT[j][:, kt * 128:(kt + 1) * 128],
                  rhs=qzb[:, 0:n], start=True, stop=True, _partial=True)
            return S

        def finalize(h, bidx):
            t0, n = BLKS[bidx]
            nq = n // 128
            ob = osb[cnt_["f"] % 2]
            fn_ = fin[cnt_["f"] % 2]
            fs = fsm[cnt_["f"] % 2]
            cnt_["f"] += 1
            for c in range(2):
                I("dve", "tensor_copy", out=ob[:, c, 0:nq, :], in_=O[c][:, 0:nq * 65].re("p (q e) -> p q e", e=65),
                  _partial=True)
            I("dve", "reciprocal", out=fs[:, 0:4], in_=ob[:, 0, :, 64])
            I("dve", "reciprocal", out=fs[:, 4:8], in_=ob[:, 1, :, 64], _partial=True)
            I("dve", "tensor_scalar", out=fs[:, 4:8], in0=fs[:, 4:8], scalar1=neg_lam, scalar2=None, op0=ALU.mult)
            for qs in range(nq):
                I("dve", "tensor_scalar", out=fn_[:, qs, :], in0=ob[:, 0, qs, 0:64], scalar1=fs[:, qs:qs + 1],
                  scalar2=None, op0=ALU.mult, _partial=True)
                I("dve", "scalar_tensor_tensor", out=fn_[:, qs, :], in0=ob[:, 1, qs, 0:64], scalar=fs[:, 4 + qs:5 + qs],
                  in1=fn_[:, qs, :], op0=ALU.mult, op1=ALU.add)
            sqv = ob[:, 0, 0:nq, 0:64]
            I("dve", "tensor_tensor", out=sqv, in0=fn_[:, 0:nq, :], in1=fn_[:, 0:nq, :], op=ALU.mult)
            I("dve", "tensor_reduce", out=fs[:, 8:8 + nq], in_=sqv, axis=AX.X, op=ALU.add)
            I("dve", "tensor_scalar", out=fs[:, 12:12 + nq], in0=fs[:, 8:8 + nq], scalar1=1.0 / 64, scalar2=EPS,
              op0=ALU.mult, op1=ALU.add)
            return (h, bidx, fn_, fs)

        def finalize2(h, bidx, fn_, fs):
            t0, n = BLKS[bidx]
            nq = n // 128
            I("act", "activation", out=fs[:, 12:12 + nq], in_=fs[:, 12:12 + nq], func=AF.Ln)
            I("act", "activation", out=fs[:, 12:12 + nq], in_=fs[:, 12:12 + nq], func=AF.Exp, scale=-0.5)
            for qs in range(nq):
                tt = t0 // 128 + qs
                I("dve", "scalar_tensor_tensor", out=ydf[:, tt, h * 64:(h + 1) * 64], in0=fn_[:, qs, :],
                  scalar=fs[:, 12 + qs:13 + qs], in1=sub_g.v, op0=ALU.mult, op1=ALU.mult, _partial=True)

        qzb_cur = prep_q(units[0])
        S_next = emit_S(units[0], units[0][1][0], qzb_cur)
        pend_f = None
        for ui, u in enumerate(units):
            ktiles, pairs, h, bidx, c = u
            t0, n = BLKS[bidx]
            nq = n // 128
            u_next = units[ui + 1] if ui + 1 < len(units) else None
            qzb_next = prep_q(u_next) if u_next is not None else None
            for pi, pair in enumerate(pairs):
                S = S_next
                if pi + 1 < len(pairs):
                    S_next = emit_S(u, pairs[pi + 1], qzb_cur)
                elif u_next is not None:
                    S_next = emit_S(u_next, u_next[1][0], qzb_next)
                npair = len(pair)
                pt_ = PT[cnt_["pt"] % 3]
                cnt_["pt"] += 1
                I("act", "activation", out=pt_[:, 0:npair, 0:n], in_=S[:, 0:npair, 0:n], func=AF.Exp, scale=sc_d)
                for uu, kt in enumerate(pair):
                    ki = 2 * pi + uu
                    I("pe", "matmul", out=OT[c][:, 0:n], lhsT=vaug[:, kt, h, :], rhs=pt_[:, uu, 0:n],
                      start=(ki == 0), stop=(ki == len(ktiles) - 1), _partial=True)
                if pend_f is not None and (pi == 4 or pi == len(pairs) - 1):
                    finalize2(*pend_f)
                    pend_f = None
            I("dve", "tensor_copy", out=osT[c][0:65, 0:n], in_=OT[c][0:65, 0:n])
            for qs in range(nq):
                I("pe", "transpose", out=O[c][:, qs * 65:(qs + 1) * 65], in_=osT[c][0:65, qs * 128:(qs + 1) * 128],
                  identity=ident_f[0:65, 0:65], _partial=True)
            if c == 1:
                pend_f = finalize(h, bidx)
            qzb_cur = qzb_next
        if pend_f is not None:
            finalize2(*pend_f)
        yT_from_tm(ydf, 2)
        P.barrier()
        A.reset(m1)

        m1 = A.mark()
        ropec = A.alloc("ropecw", [128, SEQ], F32)
        ropes = A.alloc("ropesw", [128, SEQ], F32)
        P.dma(ropec.v, c_rope[2])
        P.dma(ropes.v, c_rope[3])
        t1 = [A.alloc("wt1_%d" % i, [128, 512], F32) for i in range(4)]
        t2 = A.alloc("wt2", [128, 512], F32)
        wqT = [A.alloc("wqT%d" % j, [128, T], BF16) for j in range(2)]
        wkT = [A.alloc("wkT%d" % j, [128, T], BF16) for j in range(2)]
        for j in range(2):
            rope_proj(20 + j, 22 + j, wqT[j], ropec, ropes, t1, t2)
        for j in range(2):
            rope_proj(24 + j, 26 + j, wkT[j], ropec, ropes, t1, t2)
        wwv = load_tm_weights(28, 128, "wwv")
        vw = A.alloc("vw", [128, NT, 2, 65], BF16)
        I("pool", "memset", ap=vw.v, constant=1.0, _w=("ap",))
        for tt in range(NT):
            bidx, off = blk_of(tt)
            pp = nps()
            for kc in range(8):
                I("pe", "matmul", out=pp[:, 0:128], lhsT=hT[bidx][:, kc, off:off + 128], rhs=wwv[:, kc, :],
                  start=(kc == 0), stop=(kc == 7))
            I("dve", "tensor_copy", out=vw[:, tt, :, 0:64], in_=pp[:, 0:128].re("p (h e) -> p h e", h=2), _partial=True)
        wmask = A.alloc("wmask", [128, 3, 128], BF16)
        P.dma(wmask.v, c_wmask.v)
        esk = A.alloc("esk", [128, 4], F32)
        bcast_load(esk.v, wsink[l], 4)
        I("act", "activation", out=esk.v, in_=esk.v, func=AF.Exp)
        ywn = A.alloc("ywn", [128, NT, 256], BF16)
        PW = [A.alloc("PW%d" % i, [128, 5, 128], BF16) for i in range(3)]
        wsm = [A.alloc("wsm%d" % i, [128, 2], F32) for i in range(3)]
        sc_w = 64 ** -0.5
        cnt = 0
        qtiles = list(range(2, NT)) + ([0, 1] if need_ctx else [])
        def win_A(qt, hh, k):
            g, r = hh // 2, hh % 2
            pb = 64 * r
            if qt >= 2:
                nblk = qt - 2
                loc = [(s_, 2 + m) for s_, m in enumerate([nblk - 1, nblk, nblk + 1]) if 0 <= m < 16]
            else:
                loc = []
            chunks = [("l", s_, kt) for (s_, kt) in loc] + [("c", 3, 0), ("c", 4, 1)]
            SL = ps[k % 2]
            SC = ps[2 + k % 2]
            pw = PW[k % 3]
            for (kind, slot, kt) in chunks:
                dstp = SL[:, slot * 128:(slot + 1) * 128] if kind == "l" else SC[:, (slot - 3) * 128:(slot - 2) * 128]
                I("pe", "matmul", out=dstp, lhsT=wkT[g][pb:pb + 64, kt * 128:(kt + 1) * 128],
                  rhs=wqT[g][pb:pb + 64, qt * 128:(qt + 1) * 128], start=True, stop=True, _partial=True)
            if loc:
                s_lo, s_hi = loc[0][0], loc[-1][0] + 1
                I("act", "activation", out=pw[:, s_lo:s_hi, :], in_=SL[:, s_lo * 128:s_hi * 128].re("p (a b) -> p a b", b=128),
                  func=AF.Exp, scale=sc_w, _partial=True)
                I("dve", "tensor_tensor", out=pw[:, s_lo:s_hi, :], in0=pw[:, s_lo:s_hi, :], in1=wmask[:, s_lo:s_hi, :],
                  op=ALU.mult)
            I("act", "activation", out=pw[:, 3:5, :], in_=SC[:, 0:256].re("p (a b) -> p a b", b=128),
              func=AF.Exp, scale=sc_w, _partial=True)
            return chunks

        def win_B(qt, hh, k, chunks):
            g = hh // 2
            Ob = ps[4 + k % 2]
            pw = PW[k % 3]
            ws = wsm[k % 3]
            for ci, (kind, slot, kt) in enumerate(chunks):
                I("pe", "matmul", out=Ob[:, 0:65], lhsT=pw[:, slot, :], rhs=vw[:, kt, g, :],
                  start=(ci == 0), stop=(ci == len(chunks) - 1))
            I("dve", "tensor_tensor", out=ws[:, 0:1], in0=Ob[:, 64:65], in1=esk[:, hh:hh + 1], op=ALU.add)
            I("dve", "reciprocal", out=ws[:, 1:2], in_=ws[:, 0:1], _partial=True)
            I("dve", "tensor_scalar", out=ywn[:, qt, hh * 64:(hh + 1) * 64], in0=Ob[:, 0:64], scalar1=ws[:, 1:2],
              scalar2=None, op0=ALU.mult, _partial=True)

        items = [(qt, hh) for qt in qtiles for hh in range(4)]
        pend = None
        for k, (qt, hh) in enumerate(items):
            ch = win_A(qt, hh, k)
            if pend is not None:
                win_B(*pend)
            pend = (qt, hh, k, ch)
        win_B(*pend)
        yT_from_tm(ywn, 6)
        if debug and l == layers[0] and bi == bis[0]:
            for i, (t0, n) in enumerate(BLKS):
                P.dma(dbg["ymixT"][:, :, t0:t0 + n], ymT[i].v, partial=True)
        P.barrier()
        A.reset(m1)
        if stop_after == "mix":
            A.reset(m0)
            return

        A.reset(mW)
        tiles = list(range(2, NT)) + ([0, 1] if need_ctx else [])
        ntl = len(tiles)
        Z = [A.alloc("Z%d" % i, [128, D], F32) for i in range(ntl)]
        s1 = A.alloc("s1", [128, NT], F32)
        s2 = A.alloc("s2", [128, NT], F32)
        mean = A.alloc("mean", [128, NT], F32)
        rs = A.alloc("rs", [128, NT], F32)
        junk = A.alloc("junk", [128, D], BF16)
        mZ = A.mark()
        wo = A.alloc("wo", [128, 8, D], BF16)
        wos = [A.alloc("wos%d" % i, [128, D], F32) for i in range(2)]
        for kc in range(8):
            P.dma(wos[kc % 2].v, w_out[l, :, kc, :])
            if kc % 2 == 0:
                I("act", "activation", out=wo[:, kc, :], in_=wos[kc % 2].v, func=AF.Copy, _partial=True)
            else:
                I("dve", "tensor_copy", out=wo[:, kc, :], in_=wos[kc % 2].v, _partial=True)
        g1 = A.alloc("g1", [128, D], F32)
        g1c = A.alloc("g1c", [128, D], F32)
        bcast_load(g1.v, mod_d[l, bi, 2 * D:3 * D], D)
        bcast_load(g1c.v, mod_d[l, 2, 2 * D:3 * D], D)
        xt = [A.alloc("xt%d" % i, [128, D], F32) for i in range(2)]
        for it, tt in enumerate(tiles):
            s = it % 2
            bidx, off = blk_of(tt)
            P.dma(xt[s].v, x_src(l, bi, tt))
            pA = ps[2 * s]
            pB = ps[2 * s + 1]
            for half, pp in enumerate([pA, pB]):
                for kc in range(8):
                    I("pe", "matmul", out=pp.v, lhsT=ymT[bidx][:, kc, off:off + 128], rhs=wo[:, kc, half * 512:(half + 1) * 512],
                      start=(kc == 0), stop=(kc == 7))
            gv = g1c if tt < 2 else g1
            for half, pp in enumerate([pA, pB]):
                I("dve", "tensor_tensor", out=Z[it][:, half * 512:(half + 1) * 512], in0=pp.v,
                  in1=gv[:, half * 512:(half + 1) * 512], op=ALU.mult, _partial=True)
            I("dve", "scalar_tensor_tensor", out=Z[it].v, in0=xt[s].v, scalar=ALPHA, in1=Z[it].v, op0=ALU.mult, op1=ALU.add)
            act_sum(Z[it], junk, s1, it)
        for it in range(ntl):
            act_sum(Z[it], junk, s2, it, sq=True)
        batch_rstd2(s1, s2, mean, rs, ntl)
        P.barrier("wout1")
        A.reset(mZ)
        lg = A.alloc("lg", [128, D], F32)
        lb = A.alloc("lb", [128, D], F32)
        bcast_load(lg.v, lnv[l, 0], D)
        bcast_load(lb.v, lnv[l, 1], D)
        s1b = A.alloc("s1b", [128, NT], F32)
        s2b = A.alloc("s2b", [128, NT], F32)
        mean2 = A.alloc("mean2", [128, NT], F32)
        rs2 = A.alloc("rs2", [128, NT], F32)
        bcast_load(sh1.v, mod_d[l, bi, 3 * D:4 * D], D)
        bcast_load(sc1.v, mod_d[l, bi, 4 * D:5 * D], D)
        bcast_load(sh1c.v, mod_d[l, 2, 3 * D:4 * D], D)
        bcast_load(sc1c.v, mod_d[l, 2, 4 * D:5 * D], D)
        I("dve", "tensor_scalar_add", out=sc1.v, in0=sc1.v, scalar1=1.0)
        I("dve", "tensor_scalar_add", out=sc1c.v, in0=sc1c.v, scalar1=1.0)
        for it, tt in enumerate(tiles):
            I("dve", "scalar_tensor_tensor", out=Z[it].v, in0=Z[it].v, scalar=mean[:, it:it + 1], in1=lg.v,
              op0=ALU.subtract, op1=ALU.mult)
            I("dve", "scalar_tensor_tensor", out=Z[it].v, in0=Z[it].v, scalar=rs[:, it:it + 1], in1=lb.v,
              op0=ALU.mult, op1=ALU.add)
            if it % 4 == 3 or it == ntl - 1:
                i0 = it - (it % 4)
                cnt = it - i0 + 1
                tg = tiles[i0]
                P.dma(x1_d[bi][tg * 128:(tg + cnt) * 128, :].re("(j p) d -> p j d", p=128), A.span(Z[i0:i0 + cnt], cnt),
                      partial=True, extra_reads=Z[i0 + 1:i0 + cnt])
            act_sum(Z[it], junk, s1b, it)
        for it in range(ntl):
            act_sum(Z[it], junk, s2b, it, sq=True)
        batch_rstd2(s1b, s2b, mean2, rs2, ntl)
        hb = [A.alloc("hb%d" % i, [128, D], BF16) for i in range(2)]
        for it, tt in enumerate(tiles):
            s = it % 2
            scv, shv = (sc1c, sh1c) if tt < 2 else (sc1, sh1)
            I("dve", "scalar_tensor_tensor", out=Z[it].v, in0=Z[it].v, scalar=mean2[:, it:it + 1], in1=scv.v,
              op0=ALU.subtract, op1=ALU.mult)
            I("dve", "scalar_tensor_tensor", out=hb[s].v, in0=Z[it].v, scalar=rs2[:, it:it + 1], in1=shv.v,
              op0=ALU.mult, op1=ALU.add)
            transpose_tile(hb[s], hT, tt)
        if debug and l == layers[0] and bi == bis[0]:
            for i, (t0, n) in enumerate(BLKS):
                P.dma(dbg["h2T"][:, :, t0:t0 + n], hT[i].v, partial=True)
        P.barrier()
        A.reset(m0)

    def phase_moe(l, bi):
        last = (l == DEPTH - 1)
        need_ctx = not last
        m0 = A.mark()
        tiles = list(range(2, NT)) + ([0, 1] if need_ctx else [])
        blks = [1, 2, 3, 4] + ([0] if need_ctx else [])
        acc = [A.alloc("acc%d" % tt, [128, D], F32) for tt in range(NT)]
        comb = A.alloc("comb", [128, NT, E], F32)
        m1 = A.mark()
        rws = A.alloc("rws", [128, 8, E], F32)
        rwb = A.alloc("rwb", [128, 8, E], BF16)
        P.dma(rws.v, router_w.v)
        I("dve", "tensor_copy", out=rwb.v, in_=rws.v)
        rb = A.alloc("rb", [128, E], F32)
        bcast_load(rb.v, router_b.v, E)
        ntl = len(tiles)
        it_of = {tt: it for it, tt in enumerate(tiles)}
        W = ntl * E
        Rn = ["sc", "sel", "msk", "selm", "oh1", "oh2"]
        Rb = {k: A.alloc("r_" + k, [128, ntl, E], F32) for k in Rn}
        top1 = A.alloc("r_top1", [128, ntl, 4], F32)
        top2 = A.alloc("r_top2", [128, ntl, 4], F32)
        gs = A.alloc("r_gs", [128, ntl, 4], F32)
        rv = {k: A.alloc("r_" + k, [128, ntl], F32) for k in ["gmax", "m1", "m2", "ws", "rws"]}
        lgp = ps[0]
        for it, tt in enumerate(tiles):
            bidx, off = blk_of(tt)
            for kc in range(8):
                I("pe", "matmul", out=lgp[:, it * E:(it + 1) * E], lhsT=hT[bidx][:, kc, off:off + 128], rhs=rwb[:, kc, :],
                  start=(kc == 0), stop=(kc == 7), _partial=True)
        f3 = lambda b: b.v
        f4 = lambda b: b.v.re("p t (g e) -> p t g e", g=4)
        bc3 = lambda b: b.v.re("p (t o) -> p t o", o=1).bc([128, ntl, E])
        bc4 = lambda b: b.v.re("p t (g o) -> p t g o", o=1).bc([128, ntl, 4, 4])
        I("act", "activation", out=f3(Rb["sc"]), in_=lgp[:, 0:W].re("p (t e) -> p t e", e=E), func=AF.Sigmoid)
        I("dve", "tensor_tensor", out=f3(Rb["sel"]), in0=f3(Rb["sc"]), in1=rb.v.re("p (o e) -> p o e", o=1).bc([128, ntl, E]),
          op=ALU.add)
        I("dve", "tensor_reduce", out=top1.v, in_=f4(Rb["sel"]), axis=AX.X, op=ALU.max)
        I("dve", "tensor_tensor", out=f4(Rb["msk"]), in0=f4(Rb["sel"]), in1=bc4(top1), op=ALU.is_ge)
        I("dve", "scalar_tensor_tensor", out=f3(Rb["msk"]), in0=f3(Rb["msk"]), scalar=-BIG, in1=f3(Rb["sel"]),
          op0=ALU.mult, op1=ALU.add)
        I("dve", "tensor_reduce", out=top2.v, in_=f4(Rb["msk"]), axis=AX.X, op=ALU.max)
        I("dve", "tensor_tensor", out=gs.v, in0=top1.v, in1=top2.v, op=ALU.add)
        I("dve", "tensor_reduce", out=rv["gmax"].v, in_=gs.v, axis=AX.X, op=ALU.max)
        I("dve", "tensor_tensor", out=gs.v, in0=gs.v, in1=rv["gmax"].v.re("p (t o) -> p t o", o=1).bc([128, ntl, 4]), op=ALU.is_ge)
        I("dve", "tensor_scalar", out=gs.v, in0=gs.v, scalar1=-1.0, scalar2=BIG, op0=ALU.add, op1=ALU.mult)
        I("dve", "tensor_tensor", out=f4(Rb["selm"]), in0=f4(Rb["sel"]), in1=bc4(gs), op=ALU.add)
        I("dve", "tensor_reduce", out=rv["m1"].v, in_=f3(Rb["selm"]), axis=AX.X, op=ALU.max)
        I("dve", "tensor_tensor", out=f3(Rb["oh1"]), in0=f3(Rb["selm"]), in1=bc3(rv["m1"]), op=ALU.is_ge)
        I("dve", "scalar_tensor_tensor", out=f3(Rb["selm"]), in0=f3(Rb["oh1"]), scalar=-BIG, in1=f3(Rb["selm"]),
          op0=ALU.mult, op1=ALU.add)
        I("dve", "tensor_reduce", out=rv["m2"].v, in_=f3(Rb["selm"]), axis=AX.X, op=ALU.max)
        I("dve", "tensor_tensor", out=f3(Rb["oh2"]), in0=f3(Rb["selm"]), in1=bc3(rv["m2"]), op=ALU.is_ge)
        I("dve", "tensor_tensor", out=f3(Rb["oh1"]), in0=f3(Rb["oh1"]), in1=f3(Rb["oh2"]), op=ALU.add)
        I("dve", "tensor_tensor", out=f3(Rb["oh1"]), in0=f3(Rb["oh1"]), in1=f3(Rb["sc"]), op=ALU.mult)
        I("dve", "tensor_reduce", out=rv["ws"].v, in_=f3(Rb["oh1"]), axis=AX.X, op=ALU.add)
        I("dve", "reciprocal", out=rv["rws"].v, in_=rv["ws"].v)
        I("dve", "tensor_tensor", out=comb[:, 0:ntl, :], in0=f3(Rb["oh1"]), in1=bc3(rv["rws"]), op=ALU.mult)
        if debug and l == layers[0] and bi == bis[0]:
            P.dma(dbg["comb"].v, comb.v)
        P.barrier()
        A.reset(m1)
        wstg = [A.alloc("wstg%d" % i, [128, 2048], F32) for i in range(3)]
        wgb = [A.alloc("wgb%d" % i, [128, 8, FE], BF16) for i in range(2)]
        wub = [A.alloc("wub%d" % i, [128, 8, FE], BF16) for i in range(2)]
        wdb = [A.alloc("wdb%d" % i, [128, 4, D], BF16) for i in range(2)]
        sgb = [A.alloc("sgm%d" % i, [128, 512], BF16) for i in range(2)]
        heb = [A.alloc("he%d" % i, [128, 4, 512], BF16) for i in range(2)]
        stg_rr = [0]

        def load_expert(e):
            s = e % 2
            for (src, dst) in [(wg, wgb[s]), (wu, wub[s])]:
                for hf in range(2):
                    sg_ = wstg[stg_rr[0] % 3]
                    stg_rr[0] += 1
                    P.dma(sg_.v.re("p (a b) -> p a b", a=4), src[l, e, :, hf * 4:(hf + 1) * 4, :])
                    if e == 0:
                        I("act", "activation", out=dst[:, hf * 4:(hf + 1) * 4, :], in_=sg_.v.re("p (a b) -> p a b", a=4),
                          func=AF.Copy, _partial=True)
                    else:
                        I("pool", "tensor_copy", out=dst[:, hf * 4:(hf + 1) * 4, :], in_=sg_.v.re("p (a b) -> p a b", a=4),
                          _partial=True)
            for hf in range(2):
                sg_ = wstg[stg_rr[0] % 3]
                stg_rr[0] += 1
                P.dma(sg_.v.re("p (a b) -> p a b", a=2), wd[l, e, :, hf * 2:(hf + 1) * 2, :])
                if e == 0:
                    I("dve", "tensor_copy", out=wdb[s][:, hf * 2:(hf + 1) * 2, :], in_=sg_.v.re("p (a b) -> p a b", a=2),
                      _partial=True)
                else:
                    I("pool", "tensor_copy", out=wdb[s][:, hf * 2:(hf + 1) * 2, :], in_=sg_.v.re("p (a b) -> p a b", a=2),
                      _partial=True)

        load_expert(0)
        hcnt = 0

        def gate_up(e, bidx, he):
            s = e % 2
            t0, n = BLKS[bidx]
            for fc in range(4):
                pg = ps[(2 * fc) % 4]
                pu = ps[(2 * fc + 1) % 4]
                for kc in range(8):
                    I("pe", "matmul", out=pg[:, 0:n], lhsT=wgb[s][:, kc, fc * 128:(fc + 1) * 128], rhs=hT[bidx][:, kc, :],
                      start=(kc == 0), stop=(kc == 7))
                for kc in range(8):
                    I("pe", "matmul", out=pu[:, 0:n], lhsT=wub[s][:, kc, fc * 128:(fc + 1) * 128], rhs=hT[bidx][:, kc, :],
                      start=(kc == 0), stop=(kc == 7))
                sg_ = sgb[fc % 2]
                I("act", "activation", out=sg_[:, 0:n], in_=pg[:, 0:n], func=AF.Silu)
                I("dve", "tensor_tensor", out=he[:, fc, 0:n], in0=pu[:, 0:n], in1=sg_[:, 0:n], op=ALU.mult, _partial=True)

        def down(e, bidx, he):
            s = e % 2
            t0, n = BLKS[bidx]
            for qs in range(n // 128):
                tt = t0 // 128 + qs
                for half in range(2):
                    po = ps[4 + half]
                    for fc in range(4):
                        I("pe", "matmul", out=po.v, lhsT=he[:, fc, qs * 128:(qs + 1) * 128],
                          rhs=wdb[s][:, fc, half * 512:(half + 1) * 512], start=(fc == 0), stop=(fc == 3))
                    dst = acc[tt][:, half * 512:(half + 1) * 512]
                    cs = comb[:, it_of[tt], e:e + 1]
                    if e == 0:
                        I("dve", "tensor_scalar", out=dst, in0=po.v, scalar1=cs, scalar2=None,
                          op0=ALU.mult, _partial=True)
                    else:
                        I("dve", "scalar_tensor_tensor", out=dst, in0=po.v, scalar=cs, in1=dst,
                          op0=ALU.mult, op1=ALU.add)

        pending = None
        for e in range(E):
            for bi_, bidx in enumerate(blks):
                he = heb[hcnt % 2]
                hcnt += 1
                gate_up(e, bidx, he)
                if pending is not None:
                    down(*pending)
                pending = (e, bidx, he)
                if bi_ == 0 and e + 1 < E:
                    load_expert(e + 1)
        down(*pending)
        P.barrier()
        A.reset(m1)
        g2 = A.alloc("g2", [128, D], F32)
        g2c = A.alloc("g2c", [128, D], F32)
        lg = A.alloc("lg2", [128, D], F32)
        lb = A.alloc("lb2", [128, D], F32)
        bcast_load(g2.v, mod_d[l, bi, 5 * D:6 * D], D)
        bcast_load(g2c.v, mod_d[l, 2, 5 * D:6 * D], D)
        bcast_load(lg.v, lnv[l, 2], D)
        bcast_load(lb.v, lnv[l, 3], D)
        xg = [A.alloc("mxg%d" % i, [128, 4, D], F32) for i in range(2)]
        s1 = A.alloc("ms1", [128, NT], F32)
        s2 = A.alloc("ms2", [128, NT], F32)
        mean = A.alloc("mmean", [128, NT], F32)
        rs = A.alloc("mrs", [128, NT], F32)
        junk = A.alloc("mjunk", [128, D], BF16)
        ntl = len(tiles)
        fgroups = [(i0, min(4, ntl - i0)) for i0 in range(0, ntl, 4)]
        for gi, (i0, cnt) in enumerate(fgroups):
            tg = tiles[i0]
            xb = xg[gi % 2]
            P.dma(xb[:, 0:cnt, :], x1_d[bi][tg * 128:(tg + cnt) * 128, :].re("(j p) d -> p j d", p=128))
            for it in range(i0, i0 + cnt):
                tt = tiles[it]
                if debug and l == layers[0] and bi == bis[0]:
                    P.dma(dbg["acc"][tt * 128:(tt + 1) * 128, :], acc[tt].v, partial=True)
                gv = g2c if tt < 2 else g2
                I("dve", "tensor_tensor", out=acc[tt].v, in0=acc[tt].v, in1=gv.v, op=ALU.mult)
                I("dve", "scalar_tensor_tensor", out=acc[tt].v, in0=xb[:, it - i0, :], scalar=ALPHA, in1=acc[tt].v,
                  op0=ALU.mult, op1=ALU.add)
                act_sum(acc[tt], junk, s1, it)
        for it, tt in enumerate(tiles):
            act_sum(acc[tt], junk, s2, it, sq=True)
        batch_rstd2(s1, s2, mean, rs, ntl)
        for gi, (i0, cnt) in enumerate(fgroups):
            tg = tiles[i0]
            for it in range(i0, i0 + cnt):
                tt = tiles[it]
                I("dve", "scalar_tensor_tensor", out=acc[tt].v, in0=acc[tt].v, scalar=mean[:, it:it + 1], in1=lg.v,
                  op0=ALU.subtract, op1=ALU.mult)
                I("dve", "scalar_tensor_tensor", out=acc[tt].v, in0=acc[tt].v, scalar=rs[:, it:it + 1], in1=lb.v,
                  op0=ALU.mult, op1=ALU.add)
            src = A.span(acc[tg:tg + cnt], cnt)
            if last:
                dst = out_d[bi, (tg - 2) * 128:(tg - 2 + cnt) * 128, :]
            else:
                dst = xres_d[bi][tg * 128:(tg + cnt) * 128, :]
            P.dma(dst.re("(j p) d -> p j d", p=128), src, partial=True, extra_reads=acc[tg + 1:tg + cnt])
        P.barrier()
        A.reset(m0)

    with nc.allow_low_precision("bf16 matmul operands, fp32 accumulation"):
        with nc.Block() as block:
            phase_mod()
            for bi in bis:
                for l in layers:
                    phase_mix(l, bi)
                    if stop_after in ("lnt", "mix"):
                        continue
                    phase_moe(l, bi)
            P.barrier()

            @block.sync
            def _(e):
                P.replay("sp", e)

            @block.scalar
            def _(e):
                P.replay("act", e)

            @block.vector
            def _(e):
                P.replay("dve", e)

            @block.gpsimd
            def _(e):
                P.replay("pool", e)

            @block.tensor
            def _(e):
                P.replay("pe", e)
    nc._prog_stats = (P.n_ins, {k: len(v.ops) for k, v in P.E.items()})
    nc._marks = P.marks
    return nc


def _pack_kp(w, p=128):
    sh = w.shape
    k, n = sh[-2], sh[-1]
    w = w.reshape(sh[:-2] + (k // p, p, n))
    nd = w.ndim
    perm = list(range(nd - 3)) + [nd - 2, nd - 3, nd - 1]
    return np.ascontiguousarray(w.transpose(perm))


def prepare_inputs(x, c, ctx, c_ctx, w_mod, b_mod, w_in, w_out, conv_w, conv_b, conv_norm_g, conv_norm_b,
                   diff_lambda, diff_subln_g, win_sink, ln_mix_g, ln_mix_b, ln_ffn_g, ln_ffn_b,
                   router_w, router_bias, exp_w_gate, exp_w_up, exp_w_down):
    f = lambda a: np.ascontiguousarray(np.asarray(a, dtype=np.float32))
    x, c, ctx, c_ctx = f(x), f(c), f(ctx), f(c_ctx)
    perm = _perm_w_in()
    w_in = f(w_in)
    wx = w_in[:, :, perm]
    wx = wx.reshape(DEPTH, 8, 128, NCH, 128).transpose(0, 3, 2, 1, 4)
    shared = {
        "w_mod": _pack_kp(f(w_mod)),
        "b_mod": f(b_mod),
        "w_inx": np.ascontiguousarray(wx),
        "w_out": _pack_kp(f(w_out)),
        "conv_w": np.ascontiguousarray(f(conv_w).reshape(DEPTH, 31, 2, 128).transpose(0, 2, 3, 1)),
        "conv_v": np.ascontiguousarray(np.stack([f(conv_b), f(conv_norm_g), f(conv_norm_b)], -1).reshape(DEPTH, 2, 128, 3)),
        "dlam": f(diff_lambda).reshape(DEPTH, 128),
        "dsub": f(diff_subln_g),
        "wsink": f(win_sink),
        "lnv": np.ascontiguousarray(np.stack([f(ln_mix_g), f(ln_mix_b), f(ln_ffn_g), f(ln_ffn_b)], 1)),
        "router_w": _pack_kp(f(router_w)),
        "router_b": f(router_bias),
        "wg": _pack_kp(f(exp_w_gate)),
        "wu": _pack_kp(f(exp_w_up)),
        "wd": _pack_kp(f(exp_w_down)),
    }
    cst = _consts()
    shared["c_rope"] = cst["rope"]
    shared["c_f64"] = cst["f64"]
    shared["c_fN"] = np.ascontiguousarray(cst["fN"])
    shared["c_fC"] = np.ascontiguousarray(cst["fC"])
    shared["c_wmask"] = cst["wmask"]
    shared["c_ident"] = cst["ident"]
    shared["c_bmask"] = cst["bmask"]
    in_maps = []
    for core in range(NCORE):
        b0 = core * BPC
        m = dict(shared)
        m["x"] = x[b0:b0 + BPC]
        m["ctx"] = ctx[b0:b0 + BPC]
        cs = np.stack([c[b0], c[b0 + 1], c_ctx], -1)
        m["cT"] = np.ascontiguousarray(cs.reshape(8, 128, 3).transpose(1, 0, 2))
        in_maps.append(m)
    return in_maps


_NC_CACHE = {}


def kernel(**inputs):
    in_maps = prepare_inputs(**inputs)
    if "nc" not in _NC_CACHE:
        _NC_CACHE["nc"] = build_nc()
    nc = _NC_CACHE["nc"]
    res = run_bass_kernel_spmd(nc, in_maps, core_ids=list(range(NCORE)))
    out = np.concatenate([np.asarray(r["out"]) for r in res.results], axis=0)
    return out.astype(np.float32)
```

```python
import math
import numpy as np
import ml_dtypes
import concourse.bass as bass
import concourse.mybir as mybir
from concourse.bass_utils import run_bass_kernel_spmd

F32 = mybir.dt.float32
BF16 = mybir.dt.bfloat16
AF = mybir.ActivationFunctionType
ALU = mybir.AluOpType
AX = mybir.AxisListType

D = 1024
SEQ = 2048
CTX = 256
T = SEQ + CTX
NT = T // 128
DEPTH = 2
NCORE = 8
BPC = 2
E = 16
FE = 512
ALPHA = (2 * DEPTH) ** 0.25
EPS = 1e-5
NCH = 29
BLKS = [(0, 256), (256, 512), (768, 512), (1280, 512), (1792, 512)]
BIG = 1.0e4


class Buf:
    def __init__(self, name, full):
        self.name = name
        self.full = full
        self.w = {}
        self.wf = {}
        self.r = {}

    def __getitem__(self, key):
        return V(self.full[key], self)

    @property
    def v(self):
        return V(self.full, self)


class V:
    def __init__(self, ap, buf):
        self.ap = ap
        self.buf = buf

    def __getitem__(self, key):
        return V(self.ap[key], self.buf)

    def bc(self, shape):
        return V(self.ap.to_broadcast(shape), self.buf)

    def re(self, s, **kw):
        return V(self.ap.rearrange(s, **kw), self.buf)


class Eng:
    def __init__(self, name, semkey):
        self.name = name
        self.semkey = semkey
        self.count = 0
        self.seen = {}
        self.ops = []


class Prog:
    NDMA = 40

    def __init__(self, nc):
        self.nc = nc
        self.sems = []
        self.E = {}
        for n in ["pe", "act", "dve", "pool", "sp"]:
            self.sems.append(nc.alloc_semaphore("sem_" + n))
            self.E[n] = Eng(n, len(self.sems) - 1)
        self.dma_sems = []
        for i in range(self.NDMA):
            self.sems.append(nc.alloc_semaphore("sem_dma%d" % i))
            self.dma_sems.append(len(self.sems) - 1)
        self.dma_val = {s: 0 for s in self.dma_sems}
        self.dma_next = 0
        self.n_ins = 0

    def _deps(self, eng, reads, writes, partial, extra=()):
        deps = {}

        def need(s, v):
            if v > deps.get(s, 0):
                deps[s] = v

        for b in reads:
            for s, v in b.w.items():
                need(s, v)
        for b in writes:
            if not partial:
                for s, v in b.w.items():
                    need(s, v)
            else:
                for s, v in b.wf.items():
                    need(s, v)
            for s, v in b.r.items():
                need(s, v)
        for s, v in extra:
            need(s, v)
        if eng.name == "pe":
            deps.pop(eng.semkey, None)
        waits = []
        for s, v in deps.items():
            if eng.seen.get(s, 0) < v:
                waits.append((s, v))
                eng.seen[s] = v
        return waits

    def _update(self, tok, reads, writes, partial):
        s, v = tok
        for b in reads:
            if b.r.get(s, 0) < v:
                b.r[s] = v
        for b in writes:
            if partial:
                if b.w.get(s, 0) < v:
                    b.w[s] = v
            else:
                b.w = {s: v}
                b.wf = {s: v}
                b.r = {}

    def emit(self, en, fn, reads, writes, partial=False):
        eng = self.E[en]
        waits = self._deps(eng, reads, writes, partial)
        eng.count += 1
        tok = (eng.semkey, eng.count)
        eng.ops.append((waits, fn, (eng.semkey, 1)))
        self._update(tok, reads, writes, partial)
        self.n_ins += 1

    def I(self, en, meth, _w=("out",), _partial=False, _rw=(), **kw):
        if en == "pool" and meth == "memset":
            self.n_pool_memset = getattr(self, "n_pool_memset", 0) + 1
        reads, writes, real = [], [], {}
        for k, v in kw.items():
            if isinstance(v, V):
                if k in _w or k == "accum_out":
                    writes.append(v.buf)
                else:
                    reads.append(v.buf)
                real[k] = v.ap
            else:
                real[k] = v

        def fn(e, meth=meth, real=real):
            return getattr(e, meth)(**real)

        self.emit(en, fn, reads, writes, _partial)

    def dma(self, out, in_, q="sp", partial=False, extra_reads=()):
        eng = self.E[q]
        s = self.dma_sems[self.dma_next]
        self.dma_next = (self.dma_next + 1) % self.NDMA
        prev = self.dma_val[s]
        extra = [(s, prev)] if prev > 0 else []
        rd = [in_.buf] + list(extra_reads)
        waits = self._deps(eng, rd, [out.buf], partial, extra)
        self.dma_val[s] = prev + 16
        tok = (s, prev + 16)
        oap, iap = out.ap, in_.ap

        def fn(e):
            return e.dma_start(out=oap, in_=iap)

        eng.ops.append((waits, fn, (s, 16)))
        self._update(tok, rd, [out.buf], partial)
        self.n_ins += 1

    def barrier(self, name=None):
        if not hasattr(self, "marks"):
            self.marks = []
        self.marks.append((name, {k: len(v.ops) for k, v in self.E.items()}))
        targets = [(e.semkey, e.count) for e in self.E.values() if e.count > 0]
        targets += [(s, v) for s, v in self.dma_val.items() if v > 0]
        for eng in self.E.values():
            waits = []
            for s, v in targets:
                if s == eng.semkey and eng.name == "pe":
                    continue
                if eng.seen.get(s, 0) < v:
                    waits.append((s, v))
                    eng.seen[s] = v
            if waits:
                eng.ops.append((waits, None, None))
        if getattr(self, "marker", None) is not None:
            self.I("pool", "memset", ap=self.marker.v, constant=float(len(self.marks)), _w=("ap",))
            self.marks[-1] = (name, dict(self.marks[-1][1], memset_idx=self.n_pool_memset))

    def replay(self, en, e):
        sems = self.sems
        for waits, fn, inc in self.E[en].ops:
            for s, v in waits:
                e.wait_ge(sems[s], v)
            if fn is not None:
                ins = fn(e)
                ins.then_inc(sems[inc[0]], inc[1])


class Arena:
    def __init__(self, nc, words):
        self.t = nc.alloc_sbuf_tensor("arena", [128, words], F32)
        self.words = words
        self.off = 0

    def mark(self):
        return self.off

    def reset(self, m):
        self.off = m

    def alloc(self, name, shape, dtype):
        n = 1
        for d in shape[1:]:
            n *= d
        nb = n * (2 if dtype == BF16 else 4)
        w = (nb + 3) // 4
        assert self.off + w <= self.words, ("arena overflow", name, self.off, w, self.words)
        ap = self.t[:, self.off:self.off + w]
        self.off += w
        if dtype == BF16:
            ap = ap.bitcast(BF16)[:, 0:n]
        elif dtype != F32:
            ap = ap.bitcast(dtype)[:, 0:n]
        if len(shape) == 3:
            ap = ap.rearrange("p (a b) -> p a b", a=shape[1])
        elif len(shape) == 4:
            ap = ap.rearrange("p (a b c) -> p a b c", a=shape[1], b=shape[2])
        ap = ap[0:shape[0]]
        b = Buf(name, ap)
        b.off = self.off - w
        b.words = w
        return b

    def span(self, bufs, j):
        b0 = bufs[0]
        for i, b in enumerate(bufs):
            assert b.off == b0.off + i * b0.words
        ap = self.t[:, b0.off:b0.off + len(bufs) * b0.words].rearrange("p (j d) -> p j d", j=j)
        return V(ap, b0)


def _perm_w_in():
    o_av, o_ag, o_dq, o_dk, o_dv, o_fz, o_wq, o_wk, o_wv = 0, 256, 512, 768, 1024, 1280, 1536, 1792, 1920
    sw = lambda a: (a ^ 1)
    cols = []
    cols += [o_av + i for i in range(256)]
    cols += [o_ag + i for i in range(256)]

    def dchunks(base, swap):
        out = []
        for ci in range(3):
            for slot in range(4):
                b = 3 * ci + slot
                if slot == 3 or b >= 8:
                    b = 0
                out += [base + b * 32 + (sw(d) if swap else d) for d in range(32)]
        return out

    cols += dchunks(o_dq, False)
    cols += dchunks(o_dq, True)
    cols += dchunks(o_dk, False)
    cols += dchunks(o_dk, True)
    cols += [o_dv + i for i in range(256)]
    cols += [o_fz + i for i in range(256)]
    cols += [o_wq + i for i in range(256)]
    cols += [o_wq + sw(i) for i in range(256)]
    kd = lambda g: [o_wk + g * 64 + (i % 64) for i in range(128)]
    kds = lambda g: [o_wk + g * 64 + sw(i % 64) for i in range(128)]
    cols += kd(0) + kd(1)
    cols += kds(0) + kds(1)
    cols += [o_wv + i for i in range(128)]
    return np.array(cols, dtype=np.int64)


def _rope_tables(dh, reps):
    n_axis = dh // 4
    inv = (10000.0 ** (-np.arange(n_axis, dtype=np.float32) / n_axis)).astype(np.float32)
    t = np.arange(SEQ)
    row = (t // 64).astype(np.float32)
    col = (t % 64).astype(np.float32)
    ang = np.concatenate([row[:, None] * inv[None, :], col[:, None] * inv[None, :]], -1)
    cos = np.cos(ang).astype(np.float32)
    sin = np.sin(ang).astype(np.float32)
    ct = np.zeros((dh, SEQ), np.float32)
    st = np.zeros((dh, SEQ), np.float32)
    for d in range(dh):
        ct[d] = cos[:, d // 2]
        st[d] = (-sin[:, d // 2]) if d % 2 == 0 else sin[:, d // 2]
    return np.tile(ct, (reps, 1)).copy(), np.tile(st, (reps, 1)).copy()


def _dft_tables(n):
    k = np.arange(n, dtype=np.int64)
    ph = (np.outer(k, k) % n).astype(np.float64) * (2.0 * np.pi / n)
    return np.cos(ph), np.sin(ph)


def _consts():
    cst = {}
    cd, sd = _rope_tables(32, 4)
    cw, sw = _rope_tables(64, 2)
    cst["rope"] = np.stack([cd, sd, cw, sw], 0).astype(np.float32)
    c64, s64 = _dft_tables(64)
    cs = np.zeros((128, 256), np.float64)
    for g in range(2):
        cs[g * 64:(g + 1) * 64, g * 64:(g + 1) * 64] = c64
        cs[g * 64:(g + 1) * 64, 128 + g * 64:128 + (g + 1) * 64] = s64
    cst["f64"] = cs.astype(ml_dtypes.bfloat16)
    cN, sN = _dft_tables(SEQ)
    def pack(m, nblk, bw, ntt):
        return m.reshape(ntt, 128, nblk, bw).transpose(2, 1, 0, 3)
    cst["fN"] = np.stack([pack(cN, 4, 512, 16), pack(-sN, 4, 512, 16)], 1).astype(ml_dtypes.bfloat16)
    cC, sC = _dft_tables(CTX)
    cst["fC"] = np.stack([pack(cC, 1, 256, 2)[0], pack(-sC, 1, 256, 2)[0]], 0).astype(ml_dtypes.bfloat16)
    j = np.arange(128)[:, None]
    i = np.arange(128)[None, :]
    m = np.stack([(j >= i), np.ones((128, 128), bool), (j <= i)], 1).astype(np.float32)
    cst["wmask"] = m.astype(ml_dtypes.bfloat16)
    cst["ident"] = np.eye(128, dtype=np.float32).astype(ml_dtypes.bfloat16)
    bm = np.zeros((128, 4), np.float32)
    for s_ in range(4):
        bm[32 * s_:32 * (s_ + 1), s_] = 1.0
    cst["bmask"] = bm
    return cst


def build_nc(debug=False, layers=(0, 1), bis=(0, 1), stop_after=None):
    nc = bass.Bass("TRN2", target_bir_lowering=False)

    def din(name, shape, dt=F32):
        return Buf(name, nc.dram_tensor(name, list(shape), dt, kind="ExternalInput").ap())

    def dscr(name, shape, dt=F32, kind="Internal"):
        return Buf(name, nc.dram_tensor(name, list(shape), dt, kind=kind).ap())

    x_in = din("x", [BPC, SEQ, D])
    ctx_in = din("ctx", [BPC, CTX, D])
    cT_in = din("cT", [128, 8, 3])
    w_mod = din("w_mod", [DEPTH, 128, 8, 6 * D])
    b_mod = din("b_mod", [DEPTH, 6 * D])
    w_inx = din("w_inx", [DEPTH, NCH, 128, 8, 128])
    w_out = din("w_out", [DEPTH, 128, 8, D])
    conv_w = din("conv_w", [DEPTH, 2, 128, 31])
    conv_v = din("conv_v", [DEPTH, 2, 128, 3])
    dlam = din("dlam", [DEPTH, 128])
    dsub = din("dsub", [DEPTH, 64])
    wsink = din("wsink", [DEPTH, 4])
    lnv = din("lnv", [DEPTH, 4, D])
    router_w = din("router_w", [128, 8, E])
    router_b = din("router_b", [E])
    wg = din("wg", [DEPTH, E, 128, 8, FE])
    wu = din("wu", [DEPTH, E, 128, 8, FE])
    wd = din("wd", [DEPTH, E, 128, 4, D])
    c_rope = din("c_rope", [4, 128, SEQ])
    c_f64 = din("c_f64", [128, 256], BF16)
    c_fN = din("c_fN", [4, 2, 128, 16, 512], BF16)
    c_fC = din("c_fC", [2, 128, 2, 256], BF16)
    c_wmask = din("c_wmask", [128, 3, 128], BF16)
    c_ident = din("c_ident", [128, 128], BF16)
    c_bmask = din("c_bmask", [128, 4])

    out_d = Buf("out", nc.dram_tensor("out", [BPC, SEQ, D], F32, kind="ExternalOutput").ap())
    dk = "ExternalOutput" if debug else "Internal"
    mod_d = dscr("mod_d", [DEPTH, 3, 6 * D], kind=dk)
    xres_d = [dscr("xres_d%d" % b, [T, D], kind=dk) for b in range(BPC)]
    x1_d = [dscr("x1_d%d" % b, [T, D], kind=dk) for b in range(BPC)]
    dbg = {}
    if debug:
        dbg["hT"] = dscr("dbg_hT", [128, 8, T], BF16, kind=dk)
        dbg["ymixT"] = dscr("dbg_ymixT", [128, 8, T], BF16, kind=dk)
        dbg["h2T"] = dscr("dbg_h2T", [128, 8, T], BF16, kind=dk)
        dbg["comb"] = dscr("dbg_comb", [128, NT, E], kind=dk)
        dbg["acc"] = dscr("dbg_acc", [T, D], kind=dk)

    P = Prog(nc)
    A = Arena(nc, 53000)
    I = P.I

    psS_ap = nc.alloc_psum_tensor("psS", [128, 2048], F32)[:]
    ps = [Buf("ps%d" % i, psS_ap[:, i * 512:(i + 1) * 512]) for i in range(4)]
    ps += [Buf("ps%d" % i, nc.alloc_psum_tensor("ps%d" % i, [128, 512], F32)[:]) for i in (4, 5)]
    psS2 = [Buf("psS2_%d" % k, psS_ap[:, k * 1024:(k + 1) * 1024].rearrange("p (a b) -> p a b", a=2)) for k in range(2)]
    pst = [Buf("pst%d" % i, nc.alloc_psum_tensor("pst%d" % i, [128, 512], F32)[:]) for i in range(2)]

    def bfv(b):
        return V(b.full.bitcast(BF16), b)
    ps_rr = [0]

    def nps(group=None):
        if group is None:
            i = ps_rr[0] % 6
            ps_rr[0] += 1
            return ps[i]
        return ps[group]

    pst_rr = [0]

    def npst():
        i = pst_rr[0] % 2
        pst_rr[0] += 1
        return pst[i]

    ident = A.alloc("ident", [128, 128], BF16)
    ones_f = A.alloc("ones_f", [128, 128], F32)
    hT = [A.alloc("hT%d" % i, [128, 8, n], BF16) for i, (t0, n) in enumerate(BLKS)]
    P.dma(ident.v, c_ident.v)
    ident_f = A.alloc("ident_f", [128, 128], F32)
    I("dve", "tensor_copy", out=ident_f.v, in_=ident.v)
    I("pool", "memset", ap=ones_f.v, constant=1.0, _w=("ap",))
    P.marker = A.alloc("marker", [128, 2], F32) if debug else None
    m_persist = A.mark()

    def blk_of(tt):
        t = tt * 128
        for i, (t0, n) in enumerate(BLKS):
            if t0 <= t < t0 + n:
                return i, t - t0
        raise ValueError

    def bcast_load(dst, src_ap_1d, n):
        P.dma(dst, V(src_ap_1d.ap.partition_broadcast(128), src_ap_1d.buf))

    def phase_mod():
        m0 = A.mark()
        cT = A.alloc("cT", [128, 8, 3], F32)
        sT = A.alloc("sT", [128, 8, 3], F32)
        wst = [A.alloc("wmst%d" % i, [128, 8, 512], F32) for i in range(2)]
        bsb = A.alloc("bsb", [3, 6 * D], F32)
        msb = A.alloc("msb", [3, 6 * D], F32)
        P.dma(cT.v, cT_in.v)
        I("act", "activation", out=sT.v, in_=cT.v, func=AF.Silu)
        for l in layers:
            for r in range(3):
                P.dma(bsb[r:r + 1, :], b_mod[l:l + 1, :], partial=True)
            for cb in range(12):
                w = wst[cb % 2]
                P.dma(w.v, w_mod[l, :, :, cb * 512:(cb + 1) * 512])
                pp = nps()
                for kc in range(8):
                    I("pe", "matmul", out=pp[0:3, :], lhsT=sT[:, kc, :], rhs=w[:, kc, :],
                      start=(kc == 0), stop=(kc == 7))
                I("dve", "tensor_tensor", out=msb[:, cb * 512:(cb + 1) * 512], in0=pp[0:3, :],
                  in1=bsb[:, cb * 512:(cb + 1) * 512], op=ALU.add, _partial=True)
            P.dma(mod_d[l], msb.v)
        P.barrier()
        A.reset(m0)

    def ln_stats(xt, st, mv, rstd, nmr):
        I("dve", "bn_stats", out=st[:, 0:6], in_=xt[:, 0:512])
        I("dve", "bn_stats", out=st[:, 6:12], in_=xt[:, 512:1024], _partial=True)
        I("dve", "bn_aggr", out=mv.v, in_=st.v)
        I("dve", "tensor_scalar_add", out=rstd.v, in0=mv[:, 1:2], scalar1=EPS)
        I("act", "activation", out=rstd.v, in_=rstd.v, func=AF.Sqrt)
        I("dve", "reciprocal", out=rstd.v, in_=rstd.v)
        I("dve", "scalar_tensor_tensor", out=nmr.v, in0=mv[:, 0:1], scalar=-1.0, in1=rstd.v,
          op0=ALU.mult, op1=ALU.mult)

    def batch_rstd(mvall, rs, nm, n):
        I("dve", "tensor_scalar_add", out=rs[:, 0:n], in0=mvall[:, 0:n, 1], scalar1=EPS)
        I("act", "activation", out=rs[:, 0:n], in_=rs[:, 0:n], func=AF.Sqrt)
        I("dve", "reciprocal", out=rs[:, 0:n], in_=rs[:, 0:n])
        I("dve", "scalar_tensor_tensor", out=nm[:, 0:n], in0=mvall[:, 0:n, 0], scalar=-1.0, in1=rs[:, 0:n],
          op0=ALU.mult, op1=ALU.mult)

    def act_sum(xt, junk, s1, i, sq=False):
        xv = xt if isinstance(xt, V) else xt.v
        I("act", "activation", out=junk.v, in_=xv, func=(AF.Square if sq else AF.Identity),
          accum_out=s1[:, i:i + 1])

    def batch_rstd2(s1, s2, mean, rs, n):
        I("dve", "tensor_scalar", out=mean[:, 0:n], in0=s1[:, 0:n], scalar1=1.0 / D, scalar2=None, op0=ALU.mult)
        I("dve", "tensor_tensor", out=rs[:, 0:n], in0=mean[:, 0:n], in1=mean[:, 0:n], op=ALU.mult)
        I("dve", "scalar_tensor_tensor", out=rs[:, 0:n], in0=s2[:, 0:n], scalar=1.0 / D, in1=rs[:, 0:n],
          op0=ALU.mult, op1=ALU.subtract)
        I("dve", "tensor_scalar_add", out=rs[:, 0:n], in0=rs[:, 0:n], scalar1=EPS)
        I("act", "activation", out=rs[:, 0:n], in_=rs[:, 0:n], func=AF.Sqrt)
        I("dve", "reciprocal", out=rs[:, 0:n], in_=rs[:, 0:n])

    def tile_stats(xt, stall, mvall, i):
        I("dve", "bn_stats", out=stall[:, i, 0:6], in_=xt[:, 0:512], _partial=True)
        I("dve", "bn_stats", out=stall[:, i, 6:12], in_=xt[:, 512:1024], _partial=True)
        I("dve", "bn_aggr", out=mvall[:, i, :], in_=stall[:, i, :], _partial=True)

    def transpose_tile(src_bf, dstT_list, tt):
        pt = bfv(npst())
        for kc in range(8):
            I("pe", "transpose", out=pt[:, kc * 128:(kc + 1) * 128], in_=src_bf[:, kc * 128:(kc + 1) * 128],
              identity=ident.v, _partial=True)
        bi_, off = blk_of(tt)
        I("act", "activation", out=dstT_list[bi_][:, :, off:off + 128],
          in_=pt.re("p (a b) -> p a b", a=8), func=AF.Copy, _partial=True)

    def x_src(l, bi, tt):
        if l == 0:
            if tt < 2:
                return ctx_in[bi, tt * 128:(tt + 1) * 128, :]
            return x_in[bi, (tt - 2) * 128:(tt - 1) * 128, :]
        return xres_d[bi][tt * 128:(tt + 1) * 128, :]

    def phase_mix(l, bi):
        last = (l == DEPTH - 1)
        need_ctx = not last
        m0 = A.mark()
        sc1 = A.alloc("sc1", [128, D], F32)
        sh1 = A.alloc("sh1", [128, D], F32)
        sc1c = A.alloc("sc1c", [128, D], F32)
        sh1c = A.alloc("sh1c", [128, D], F32)
        ymT = [A.alloc("ymT%d" % i, [128, 8, n], BF16) for i, (t0, n) in enumerate(BLKS)]
        mW = A.mark()
        wst = [A.alloc("wst%d" % i, [128, 8, 128], F32) for i in range(2)]
        wbf = [A.alloc("wbf%d" % i, [128, 8, 128], BF16) for i in range(2)]
        order = [18, 19, 4, 7, 10, 13, 5, 8, 11, 14, 6, 9, 12, 15, 20, 22, 21, 23, 24, 26, 25, 27]
        st_ = {"ptr": 0, "loaded": {}}

        def _load(i):
            c = order[i]
            s = i % 2
            P.dma(wst[s].v, w_inx[l, c])
            I("pool", "tensor_copy", out=wbf[s].v, in_=wst[s].v)
            st_["loaded"][i] = wbf[s]

        _load(0)

        def wchunk(c):
            i = st_["ptr"]
            assert order[i] == c, (order[i], c)
            st_["ptr"] += 1
            if i + 1 < len(order):
                _load(i + 1)
            return st_["loaded"][i]

        def proj_fm(c, blks, consume):
            w = wchunk(c)
            for bidx in blks:
                t0, n = BLKS[bidx]
                pp = nps()
                for kc in range(8):
                    I("pe", "matmul", out=pp[:, 0:n], lhsT=w[:, kc, :], rhs=hT[bidx][:, kc, :],
                      start=(kc == 0), stop=(kc == 7))
                consume(pp[:, 0:n], bidx, t0, n)

        bcast_load(sh1.v, mod_d[l, bi, 0:D], D)
        bcast_load(sc1.v, mod_d[l, bi, D:2 * D], D)
        bcast_load(sh1c.v, mod_d[l, 2, 0:D], D)
        bcast_load(sc1c.v, mod_d[l, 2, D:2 * D], D)
        I("dve", "tensor_scalar_add", out=sc1.v, in0=sc1.v, scalar1=1.0)
        I("dve", "tensor_scalar_add", out=sc1c.v, in0=sc1c.v, scalar1=1.0)

        m1 = A.mark()
        groups = [(0, 2)] + [(2 + 4 * g_, 4) for g_ in range(4)]
        XG = [A.alloc("XG%d" % gi, [128, cnt, D], F32) for gi, (tg, cnt) in enumerate(groups)]

        def Xv(tt):
            for gi, (tg, cnt) in enumerate(groups):
                if tg <= tt < tg + cnt:
                    return XG[gi][:, tt - tg, :]

        def x_src_group(tg, cnt):
            if l == 0:
                src = ctx_in[bi, 0:256, :] if tg == 0 else x_in[bi, (tg - 2) * 128:(tg - 2 + cnt) * 128, :]
            else:
                src = xres_d[bi][tg * 128:(tg + cnt) * 128, :]
            return src.re("(j p) d -> p j d", p=128)

        hb = [A.alloc("hb%d" % i, [128, D], BF16) for i in range(2)]
        s1 = A.alloc("s1", [128, NT], F32)
        s2 = A.alloc("s2", [128, NT], F32)
        mean = A.alloc("mean", [128, NT], F32)
        rs = A.alloc("rs", [128, NT], F32)
        junk = A.alloc("junk", [128, D], BF16)
        for gi, (tg, cnt) in enumerate(groups):
            P.dma(XG[gi].v, x_src_group(tg, cnt))
        for tt in range(NT):
            act_sum(Xv(tt), junk, s1, tt)
        for tt in range(NT):
            act_sum(Xv(tt), junk, s2, tt, sq=True)
        batch_rstd2(s1, s2, mean, rs, NT)
        for tt in range(NT):
            s = tt % 2
            scv, shv = (sc1c, sh1c) if tt < 2 else (sc1, sh1)
            I("dve", "scalar_tensor_tensor", out=Xv(tt), in0=Xv(tt), scalar=mean[:, tt:tt + 1], in1=scv.v,
              op0=ALU.subtract, op1=ALU.mult, _partial=True)
            I("dve", "scalar_tensor_tensor", out=hb[s].v, in0=Xv(tt), scalar=rs[:, tt:tt + 1], in1=shv.v,
              op0=ALU.mult, op1=ALU.add)
            transpose_tile(hb[s], hT, tt)
        if debug and l == layers[0] and bi == bis[0]:
            for i, (t0, n) in enumerate(BLKS):
                P.dma(dbg["hT"][:, :, t0:t0 + n], hT[i].v, partial=True)
        P.barrier()
        A.reset(m1)
        if stop_after == "lnt":
            A.reset(m0)
            return

        segs = [("lat", [1, 2, 3, 4], 256, SEQ)] + ([("ctx", [0], 0, CTX)] if need_ctx else [])

        m1 = A.mark()
        cw = [A.alloc("cw%d" % c, [128, 31], F32) for c in range(2)]
        cv = [A.alloc("cv%d" % c, [128, 3], F32) for c in range(2)]
        for c in range(2):
            P.dma(cw[c].v, conv_w[l, c])
            P.dma(cv[c].v, conv_v[l, c])
        gb = [A.alloc("gb%d" % c, [128, SEQ + 30], BF16) for c in range(2)]
        dg = [A.alloc("dg%d" % c, [128, 31, 128], BF16) for c in range(2)]
        for c in range(2):
            for j in range(31):
                I("dve", "tensor_scalar", out=dg[c][:, j, :], in0=ident.v, scalar1=cw[c][:, j:j + 1],
                  scalar2=None, op0=ALU.mult, _partial=True)
        ya = [A.alloc("ya%d" % c, [128, SEQ], F32) for c in range(2)]
        sgb = [A.alloc("sgb%d" % i, [128, 512], F32) for i in range(2)]
        sq = A.alloc("sq", [128, 512], F32)
        mean_t = A.alloc("mean_t", [128, 512], F32)
        rstd_t = A.alloc("rstd_t", [128, 512], F32)
        tmp_t = A.alloc("tmp_t", [128, 512], F32)
        wkeep = [A.alloc("wkeep%d" % i, [128, 8, 128], BF16) for i in range(4)]
        wks = [A.alloc("wks%d" % i, [128, 8, 128], F32) for i in range(2)]
        for i, c in enumerate([0, 2, 1, 3]):
            P.dma(wks[i % 2].v, w_inx[l, c])
            I("act", "activation", out=wkeep[i].v, in_=wks[i % 2].v, func=AF.Copy)
        for (sname, blks, s0, sn) in segs:
            for c in range(2):
                I("pool", "memset", ap=gb[c][:, 0:15], constant=0.0, _w=("ap",), _partial=True)
                I("pool", "memset", ap=gb[c][:, 15 + sn:30 + sn], constant=0.0, _w=("ap",), _partial=True)
                for bidx in blks:
                    t0, n = BLKS[bidx]
                    pv = nps()
                    pg = nps()
                    for kc in range(8):
                        I("pe", "matmul", out=pv[:, 0:n], lhsT=wkeep[2 * c][:, kc, :], rhs=hT[bidx][:, kc, :],
                          start=(kc == 0), stop=(kc == 7))
                    for kc in range(8):
                        I("pe", "matmul", out=pg[:, 0:n], lhsT=wkeep[2 * c + 1][:, kc, :], rhs=hT[bidx][:, kc, :],
                          start=(kc == 0), stop=(kc == 7))
                    sg = sgb[bidx % 2]
                    I("act", "activation", out=sg[:, 0:n], in_=pg[:, 0:n], func=AF.Sigmoid)
                    o = 15 + t0 - s0
                    I("dve", "tensor_tensor", out=gb[c][:, o:o + n], in0=pv[:, 0:n], in1=sg[:, 0:n], op=ALU.mult,
                      _partial=True)
                for bidx in blks:
                    t0, n = BLKS[bidx]
                    o = t0 - s0
                    pc = nps()
                    for j in range(31):
                        I("pe", "matmul", out=pc[:, 0:n], lhsT=dg[c][:, j, :], rhs=gb[c][:, o + j:o + j + n],
                          start=(j == 0), stop=(j == 30))
                    I("act", "activation", out=ya[c][:, o:o + n], in_=pc[:, 0:n], func=AF.Identity, bias=cv[c][:, 0:1],
                      _partial=True)
            for bidx in blks:
                t0, n = BLKS[bidx]
                o = t0 - s0
                p1 = nps()
                p2 = nps()
                for c in range(2):
                    I("pe", "matmul", out=p1[:, 0:n], lhsT=ones_f.v, rhs=ya[c][:, o:o + n], start=(c == 0), stop=(c == 1))
                for c in range(2):
                    I("act", "activation", out=sq[:, 0:n], in_=ya[c][:, o:o + n], func=AF.Square)
                    I("pe", "matmul", out=p2[:, 0:n], lhsT=ones_f.v, rhs=sq[:, 0:n], start=(c == 0), stop=(c == 1))
                I("dve", "tensor_scalar", out=mean_t[:, 0:n], in0=p1[:, 0:n], scalar1=1.0 / 256, scalar2=None, op0=ALU.mult)
                I("dve", "tensor_tensor", out=tmp_t[:, 0:n], in0=mean_t[:, 0:n], in1=mean_t[:, 0:n], op=ALU.mult)
                I("dve", "scalar_tensor_tensor", out=tmp_t[:, 0:n], in0=p2[:, 0:n], scalar=1.0 / 256, in1=tmp_t[:, 0:n],
                  op0=ALU.mult, op1=ALU.subtract)
                I("dve", "tensor_scalar_add", out=rstd_t[:, 0:n], in0=tmp_t[:, 0:n], scalar1=EPS)
                I("act", "activation", out=rstd_t[:, 0:n], in_=rstd_t[:, 0:n], func=AF.Sqrt)
                I("dve", "reciprocal", out=rstd_t[:, 0:n], in_=rstd_t[:, 0:n])
                for c in range(2):
                    I("dve", "tensor_tensor", out=tmp_t[:, 0:n], in0=ya[c][:, o:o + n], in1=mean_t[:, 0:n], op=ALU.subtract)
                    I("dve", "tensor_tensor", out=tmp_t[:, 0:n], in0=tmp_t[:, 0:n], in1=rstd_t[:, 0:n], op=ALU.mult)
                    I("act", "activation", out=ymT[bidx][:, c, :], in_=tmp_t[:, 0:n], func=AF.Silu,
                      bias=cv[c][:, 2:3], scale=cv[c][:, 1:2], _partial=True)
        P.barrier()
        A.reset(m1)

        m1 = A.mark()
        f64 = A.alloc("f64", [128, 256], BF16)
        P.dma(f64.v, c_f64.v)
        zT = [A.alloc("zT%d" % c, [128, T], BF16) for c in range(2)]
        AB = A.alloc("AB", [128, NT, 2, 256], BF16)
        ftabs = [[A.alloc("ftab%d_%d" % (k, i), [128, 16, 512], BF16) for i in range(2)] for k in range(2)]
        all_blks = [1, 2, 3, 4] + ([0] if need_ctx else [])
        for c in range(2):
            def cons(pv, bidx, t0, n, c=c):
                I("act", "activation", out=zT[c][:, t0:t0 + n], in_=pv, func=AF.Copy, _partial=True)
            proj_fm(18 + c, all_blks, cons)
        tiles = list(range(2, NT)) + ([0, 1] if need_ctx else [])
        for tt in tiles:
            for c in range(2):
                pp = nps()
                I("pe", "matmul", out=pp[:, 0:256], lhsT=zT[c][:, tt * 128:(tt + 1) * 128], rhs=f64.v, start=True, stop=True)
                I("dve", "tensor_copy", out=AB[:, tt, c, :], in_=pp[:, 0:256], _partial=True)
        for (sname, blks, s0, sn) in segs:
            ntt = sn // 128
            tt0 = s0 // 128
            scale = 1.0 / math.sqrt(sn * 64.0)
            def load_tab(kb_):
                ft = ftabs[kb_ % 2]
                if sname == "lat":
                    for j in range(2):
                        P.dma(ft[j].v, c_fN[kb_, j])
                else:
                    for j in range(2):
                        P.dma(ft[j][:, 0:2, 0:256], c_fC[j])

            load_tab(0)
            for kb, bidx in enumerate(blks):
                t0, n = BLKS[bidx]
                if kb + 1 < len(blks):
                    load_tab(kb + 1)
                ftab = ftabs[kb % 2]
                for c in range(2):
                    pp = nps()
                    for ti in range(ntt):
                        for j in range(2):
                            I("pe", "matmul", out=pp[:, 0:n], lhsT=AB[:, tt0 + ti, c, j * 128:(j + 1) * 128],
                              rhs=ftab[j][:, ti, 0:n], start=(ti == 0 and j == 0), stop=(ti == ntt - 1 and j == 1))
                    I("act", "activation", out=ymT[bidx][:, 4 + c, :], in_=pp[:, 0:n], func=AF.Copy, scale=scale,
                      _partial=True)
        P.barrier()
        A.reset(m1)

        def rope_proj(c_raw, c_sw, dst, tab_c, tab_s, t1, t2):
            res = {}

            def cons_raw(pv, bidx, t0, n):
                if bidx == 0:
                    I("act", "activation", out=dst[:, t0:t0 + n], in_=pv, func=AF.Copy, _partial=True)
                else:
                    I("dve", "tensor_tensor", out=res[bidx][:, 0:n], in0=pv, in1=tab_c[:, t0 - 256:t0 - 256 + n], op=ALU.mult)

            def cons_sw(pv, bidx, t0, n):
                I("dve", "tensor_tensor", out=t2[:, 0:n], in0=pv, in1=tab_s[:, t0 - 256:t0 - 256 + n], op=ALU.mult)
                I("dve", "tensor_tensor", out=dst[:, t0:t0 + n], in0=res[bidx][:, 0:n], in1=t2[:, 0:n], op=ALU.add,
                  _partial=True)

            for bidx in [1, 2, 3, 4]:
                res[bidx] = t1[bidx - 1]
            proj_fm(c_raw, [0, 1, 2, 3, 4], cons_raw)
            proj_fm(c_sw, [1, 2, 3, 4], cons_sw)

        def load_tm_weights(c0, ncols, name):
            wt = A.alloc(name, [128, 8, ncols], BF16)
            for j in range(ncols // 128):
                s = j % 2
                P.dma(wst[s].v, w_inx[l, c0 + j])
                I("act", "activation", out=wt[:, :, j * 128:(j + 1) * 128], in_=wst[s].v, func=AF.Copy, _partial=True)
            return wt

        def yT_from_tm(ytm, chunk0):
            tl = list(range(2, NT)) + ([0, 1] if need_ctx else [])
            for tt in tl:
                pt = bfv(npst())
                for j in range(2):
                    I("pe", "transpose", out=pt[:, j * 128:(j + 1) * 128], in_=ytm[:, tt, j * 128:(j + 1) * 128],
                      identity=ident.v, _partial=True)
                bi_, off = blk_of(tt)
                I("act", "activation", out=ymT[bi_][:, chunk0:chunk0 + 2, off:off + 128],
                  in_=pt[:, 0:256].re("p (a b) -> p a b", a=2), func=AF.Copy, _partial=True)

        m1 = A.mark()
        lam_init = 0.8 - 0.6 * math.exp(-0.3 * l)
        qT = [A.alloc("qT%d" % j, [128, T], BF16) for j in range(3)]
        kT = [A.alloc("kT%d" % j, [128, T], BF16) for j in range(3)]
        vaug = A.alloc("vaug", [128, NT, 4, 128], BF16)
        ydf = A.alloc("ydf", [128, NT, 256], BF16)
        PT = [A.alloc("PT%d" % i, [128, 2, 512], BF16) for i in range(4)]
        osT = [A.alloc("osT%d" % i, [128, 512], F32) for i in range(2)]
        osb = [A.alloc("osb%d" % i, [128, 2, 4, 65], F32) for i in range(2)]
        fin = [A.alloc("fin%d" % i, [128, 4, 64], F32) for i in range(2)]
        fsm = [A.alloc("fsm%d" % i, [128, 16], F32) for i in range(2)]
        qz = [A.alloc("qz%d" % i, [128, 512], BF16) for i in range(2)]
        bmask = A.alloc("bmask", [128, 4], F32)
        P.dma(bmask.v, c_bmask.v)
        lv = A.alloc("lv", [128, 128], F32)
        lsm = A.alloc("lsm", [128, 8], F32)
        lpr = A.alloc("lpr", [128, 64], F32)
        sub_g = A.alloc("sub_g", [128, 64], F32)
        m_prep = A.mark()
        ropec = A.alloc("ropec", [128, SEQ], F32)
        ropes = A.alloc("ropes", [128, SEQ], F32)
        P.dma(ropec.v, c_rope[0])
        P.dma(ropes.v, c_rope[1])
        t1 = [A.alloc("rt1_%d" % i, [128, 512], F32) for i in range(4)]
        t2 = A.alloc("rt2", [128, 512], F32)
        for j in range(3):
            rope_proj(4 + j, 7 + j, qT[j], ropec, ropes, t1, t2)
            rope_proj(10 + j, 13 + j, kT[j], ropec, ropes, t1, t2)
        wdv = load_tm_weights(16, 256, "wdv")
        I("pool", "memset", ap=vaug.v, constant=1.0, _w=("ap",))
        for tt in range(NT):
            bidx, off = blk_of(tt)
            pp = nps()
            for kc in range(8):
                I("pe", "matmul", out=pp[:, 0:256], lhsT=hT[bidx][:, kc, off:off + 128], rhs=wdv[:, kc, :],
                  start=(kc == 0), stop=(kc == 7))
            I("dve", "tensor_copy", out=vaug[:, tt, :, 0:64], in_=pp[:, 0:256].re("p (h e) -> p h e", h=4), _partial=True)
        bcast_load(lv.v, dlam[l], 128)
        I("dve", "tensor_tensor", out=lpr[:, 0:32], in0=lv[:, 0:32], in1=lv[:, 32:64], op=ALU.mult)
        I("dve", "tensor_tensor", out=lpr[:, 32:64], in0=lv[:, 64:96], in1=lv[:, 96:128], op=ALU.mult, _partial=True)
        I("dve", "reduce_sum", out=lsm[:, 0:2], in_=lpr.v.re("p (a b) -> p a b", a=2), axis=AX.X)
        I("act", "activation", out=lsm[:, 2:4], in_=lsm[:, 0:2], func=AF.Exp, _partial=True)
        I("dve", "tensor_tensor", out=lsm[:, 4:5], in0=lsm[:, 3:4], in1=lsm[:, 2:3], op=ALU.subtract, _partial=True)
        I("dve", "tensor_scalar_add", out=lsm[:, 5:6], in0=lsm[:, 4:5], scalar1=-lam_init, _partial=True)
        neg_lam = lsm[:, 5:6]
        bcast_load(sub_g.v, dsub[l], 64)
        I("dve", "tensor_scalar", out=sub_g.v, in0=sub_g.v, scalar1=(1.0 - lam_init), scalar2=None, op0=ALU.mult)
        pt_rr = 0
        sc_d = 32 ** -0.5
        fcnt = 0
        P.barrier("diff_prep")
        A.reset(m_prep)
        OT = [ps[4], ps[5]]
        O = [pst[0], pst[1]]
        units = []
        for (sname, blks, s0, sn) in segs:
            ktiles = list(range(NT)) if sname == "lat" else [0, 1]
            pairs = [ktiles[i:i + 2] for i in range(0, len(ktiles), 2)]
            for h in range(4):
                for bidx in blks:
                    for c in range(2):
                        units.append((ktiles, pairs, h, bidx, c))
        cnt_ = {"s": 0, "qz": 0, "pt": 0, "f": 0}

        def prep_q(u):
            ktiles, pairs, h, bidx, c = u
            t0, n = BLKS[bidx]
            j = (2 * h + c) // 3
            slot = (2 * h + c) % 3
            qzb = qz[cnt_["qz"] % 2]
            cnt_["qz"] += 1
            I("dve", "tensor_scalar", out=qzb[:, 0:n], in0=qT[j][:, t0:t0 + n], scalar1=bmask[:, slot:slot + 1],
              scalar2=None, op0=ALU.mult)
            return qzb

        def emit_S(u, pair, qzb):
            ktiles, pairs, h, bidx, c = u
            t0, n = BLKS[bidx]
            j = (2 * h + c) // 3
            S = psS2[cnt_["s"] % 2]
            cnt_["s"] += 1
            for uu, kt in enumerate(pair):
                I("pe", "matmul", out=S[:, uu, 0:n], lhsT=kT[j][:, kt * 128:(kt + 1) * 128],
                  rhs=qzb[:, 0:n], start=True, stop=True, _partial=True)
            return S

        def finalize(h, bidx):
            t0, n = BLKS[bidx]
            nq = n // 128
            ob = osb[cnt_["f"] % 2]
            fn_ = fin[cnt_["f"] % 2]
            fs = fsm[cnt_["f"] % 2]
            cnt_["f"] += 1
            for c in range(2):
                I("dve", "tensor_copy", out=ob[:, c, 0:nq, :], in_=O[c][:, 0:nq * 65].re("p (q e) -> p q e", e=65),
                  _partial=True)
            I("dve", "reciprocal", out=fs[:, 0:4], in_=ob[:, 0, :, 64])
            I("dve", "reciprocal", out=fs[:, 4:8], in_=ob[:, 1, :, 64], _partial=True)
            I("dve", "tensor_scalar", out=fs[:, 4:8], in0=fs[:, 4:8], scalar1=neg_lam, scalar2=None, op0=ALU.mult)
            for qs in range(nq):
                I("dve", "tensor_scalar", out=fn_[:, qs, :], in0=ob[:, 0, qs, 0:64], scalar1=fs[:, qs:qs + 1],
                  scalar2=None, op0=ALU.mult, _partial=True)
                I("dve", "scalar_tensor_tensor", out=fn_[:, qs, :], in0=ob[:, 1, qs, 0:64], scalar=fs[:, 4 + qs:5 + qs],
                  in1=fn_[:, qs, :], op0=ALU.mult, op1=ALU.add)
            sqv = ob[:, 0, 0:nq, 0:64]
            I("dve", "tensor_tensor", out=sqv, in0=fn_[:, 0:nq, :], in1=fn_[:, 0:nq, :], op=ALU.mult)
            I("dve", "tensor_reduce", out=fs[:, 8:8 + nq], in_=sqv, axis=AX.X, op=ALU.add)
            I("dve", "tensor_scalar", out=fs[:, 12:12 + nq], in0=fs[:, 8:8 + nq], scalar1=1.0 / 64, scalar2=EPS,
              op0=ALU.mult, op1=ALU.add)
            return (h, bidx, fn_, fs)

        def finalize2(h, bidx, fn_, fs):
            t0, n = BLKS[bidx]
            nq = n // 128
            I("act", "activation", out=fs[:, 12:12 + nq], in_=fs[:, 12:12 + nq], func=AF.Ln)
            I("act", "activation", out=fs[:, 12:12 + nq], in_=fs[:, 12:12 + nq], func=AF.Exp, scale=-0.5)
            for qs in range(nq):
                tt = t0 // 128 + qs
                I("dve", "scalar_tensor_tensor", out=ydf[:, tt, h * 64:(h + 1) * 64], in0=fn_[:, qs, :],
                  scalar=fs[:, 12 + qs:13 + qs], in1=sub_g.v, op0=ALU.mult, op1=ALU.mult, _partial=True)

        qzb_cur = prep_q(units[0])
        S_next = emit_S(units[0], units[0][1][0], qzb_cur)
        pend_f = None
        for ui, u in enumerate(units):
            ktiles, pairs, h, bidx, c = u
            t0, n = BLKS[bidx]
            nq = n // 128
            u_next = units[ui + 1] if ui + 1 < len(units) else None
            qzb_next = prep_q(u_next) if u_next is not None else None
            for pi, pair in enumerate(pairs):
                S = S_next
                if pi + 1 < len(pairs):
                    S_next = emit_S(u, pairs[pi + 1], qzb_cur)
                elif u_next is not None:
                    S_next = emit_S(u_next, u_next[1][0], qzb_next)
                npair = len(pair)
                pt_ = PT[cnt_["pt"] % 4]
                cnt_["pt"] += 1
                I("act", "activation", out=pt_[:, 0:npair, 0:n], in_=S[:, 0:npair, 0:n], func=AF.Exp, scale=sc_d)
                for uu, kt in enumerate(pair):
                    ki = 2 * pi + uu
                    I("pe", "matmul", out=OT[c][:, 0:n], lhsT=vaug[:, kt, h, :], rhs=pt_[:, uu, 0:n],
                      start=(ki == 0), stop=(ki == len(ktiles) - 1), _partial=True)
                if pend_f is not None and (pi == 4 or pi == len(pairs) - 1):
                    finalize2(*pend_f)
                    pend_f = None
            I("dve", "tensor_copy", out=osT[c][0:65, 0:n], in_=OT[c][0:65, 0:n])
            for qs in range(nq):
                I("pe", "transpose", out=O[c][:, qs * 65:(qs + 1) * 65], in_=osT[c][0:65, qs * 128:(qs + 1) * 128],
                  identity=ident_f[0:65, 0:65], _partial=True)
            if c == 1:
                pend_f = finalize(h, bidx)
            qzb_cur = qzb_next
        if pend_f is not None:
            finalize2(*pend_f)
        yT_from_tm(ydf, 2)
        P.barrier()
        A.reset(m1)

        m1 = A.mark()
        ropec = A.alloc("ropecw", [128, SEQ], F32)
        ropes = A.alloc("ropesw", [128, SEQ], F32)
        P.dma(ropec.v, c_rope[2])
        P.dma(ropes.v, c_rope[3])
        t1 = [A.alloc("wt1_%d" % i, [128, 512], F32) for i in range(4)]
        t2 = A.alloc("wt2", [128, 512], F32)
        wqT = [A.alloc("wqT%d" % j, [128, T], BF16) for j in range(2)]
        wkT = [A.alloc("wkT%d" % j, [128, T], BF16) for j in range(2)]
        for j in range(2):
            rope_proj(20 + j, 22 + j, wqT[j], ropec, ropes, t1, t2)
        for j in range(2):
            rope_proj(24 + j, 26 + j, wkT[j], ropec, ropes, t1, t2)
        wwv = load_tm_weights(28, 128, "wwv")
        vw = A.alloc("vw", [128, NT, 2, 65], BF16)
        I("pool", "memset", ap=vw.v, constant=1.0, _w=("ap",))
        for tt in range(NT):
            bidx, off = blk_of(tt)
            pp = nps()
            for kc in range(8):
                I("pe", "matmul", out=pp[:, 0:128], lhsT=hT[bidx][:, kc, off:off + 128], rhs=wwv[:, kc, :],
                  start=(kc == 0), stop=(kc == 7))
            I("dve", "tensor_copy", out=vw[:, tt, :, 0:64], in_=pp[:, 0:128].re("p (h e) -> p h e", h=2), _partial=True)
        wmask = A.alloc("wmask", [128, 3, 128], BF16)
        P.dma(wmask.v, c_wmask.v)
        esk = A.alloc("esk", [128, 4], F32)
        bcast_load(esk.v, wsink[l], 4)
        I("act", "activation", out=esk.v, in_=esk.v, func=AF.Exp)
        ywn = A.alloc("ywn", [128, NT, 256], BF16)
        PW = [A.alloc("PW%d" % i, [128, 5, 128], BF16) for i in range(3)]
        wsm = [A.alloc("wsm%d" % i, [128, 2], F32) for i in range(3)]
        sc_w = 64 ** -0.5
        cnt = 0
        qtiles = list(range(2, NT)) + ([0, 1] if need_ctx else [])
        def win_A(qt, hh, k):
            g, r = hh // 2, hh % 2
            pb = 64 * r
            if qt >= 2:
                nblk = qt - 2
                loc = [(s_, 2 + m) for s_, m in enumerate([nblk - 1, nblk, nblk + 1]) if 0 <= m < 16]
            else:
                loc = []
            chunks = [("l", s_, kt) for (s_, kt) in loc] + [("c", 3, 0), ("c", 4, 1)]
            SL = ps[k % 2]
            SC = ps[2 + k % 2]
            pw = PW[k % 3]
            for (kind, slot, kt) in chunks:
                dstp = SL[:, slot * 128:(slot + 1) * 128] if kind == "l" else SC[:, (slot - 3) * 128:(slot - 2) * 128]
                I("pe", "matmul", out=dstp, lhsT=wkT[g][pb:pb + 64, kt * 128:(kt + 1) * 128],
                  rhs=wqT[g][pb:pb + 64, qt * 128:(qt + 1) * 128], start=True, stop=True, _partial=True)
            if loc:
                s_lo, s_hi = loc[0][0], loc[-1][0] + 1
                I("act", "activation", out=pw[:, s_lo:s_hi, :], in_=SL[:, s_lo * 128:s_hi * 128].re("p (a b) -> p a b", b=128),
                  func=AF.Exp, scale=sc_w, _partial=True)
                I("dve", "tensor_tensor", out=pw[:, s_lo:s_hi, :], in0=pw[:, s_lo:s_hi, :], in1=wmask[:, s_lo:s_hi, :],
                  op=ALU.mult)
            I("act", "activation", out=pw[:, 3:5, :], in_=SC[:, 0:256].re("p (a b) -> p a b", b=128),
              func=AF.Exp, scale=sc_w, _partial=True)
            return chunks

        def win_B(qt, hh, k, chunks):
            g = hh // 2
            Ob = ps[4 + k % 2]
            pw = PW[k % 3]
            ws = wsm[k % 3]
            for ci, (kind, slot, kt) in enumerate(chunks):
                I("pe", "matmul", out=Ob[:, 0:65], lhsT=pw[:, slot, :], rhs=vw[:, kt, g, :],
                  start=(ci == 0), stop=(ci == len(chunks) - 1))
            I("dve", "tensor_tensor", out=ws[:, 0:1], in0=Ob[:, 64:65], in1=esk[:, hh:hh + 1], op=ALU.add)
            I("dve", "reciprocal", out=ws[:, 1:2], in_=ws[:, 0:1], _partial=True)
            I("dve", "tensor_scalar", out=ywn[:, qt, hh * 64:(hh + 1) * 64], in0=Ob[:, 0:64], scalar1=ws[:, 1:2],
              scalar2=None, op0=ALU.mult, _partial=True)

        items = [(qt, hh) for qt in qtiles for hh in range(4)]
        pend = None
        for k, (qt, hh) in enumerate(items):
            ch = win_A(qt, hh, k)
            if pend is not None:
                win_B(*pend)
            pend = (qt, hh, k, ch)
        win_B(*pend)
        yT_from_tm(ywn, 6)
        if debug and l == layers[0] and bi == bis[0]:
            for i, (t0, n) in enumerate(BLKS):
                P.dma(dbg["ymixT"][:, :, t0:t0 + n], ymT[i].v, partial=True)
        P.barrier()
        A.reset(m1)
        if stop_after == "mix":
            A.reset(m0)
            return

        A.reset(mW)
        tiles = list(range(2, NT)) + ([0, 1] if need_ctx else [])
        ntl = len(tiles)
        Z = [A.alloc("Z%d" % i, [128, D], F32) for i in range(ntl)]
        s1 = A.alloc("s1", [128, NT], F32)
        s2 = A.alloc("s2", [128, NT], F32)
        mean = A.alloc("mean", [128, NT], F32)
        rs = A.alloc("rs", [128, NT], F32)
        junk = A.alloc("junk", [128, D], BF16)
        mZ = A.mark()
        wo = A.alloc("wo", [128, 8, D], BF16)
        wos = [A.alloc("wos%d" % i, [128, D], F32) for i in range(2)]
        for kc in range(8):
            P.dma(wos[kc % 2].v, w_out[l, :, kc, :])
            if kc % 2 == 0:
                I("act", "activation", out=wo[:, kc, :], in_=wos[kc % 2].v, func=AF.Copy, _partial=True)
            else:
                I("dve", "tensor_copy", out=wo[:, kc, :], in_=wos[kc % 2].v, _partial=True)
        g1 = A.alloc("g1", [128, D], F32)
        g1c = A.alloc("g1c", [128, D], F32)
        bcast_load(g1.v, mod_d[l, bi, 2 * D:3 * D], D)
        bcast_load(g1c.v, mod_d[l, 2, 2 * D:3 * D], D)
        xt = [A.alloc("xt%d" % i, [128, D], F32) for i in range(2)]
        for it, tt in enumerate(tiles):
            s = it % 2
            bidx, off = blk_of(tt)
            P.dma(xt[s].v, x_src(l, bi, tt))
            pA = ps[2 * s]
            pB = ps[2 * s + 1]
            for half, pp in enumerate([pA, pB]):
                for kc in range(8):
                    I("pe", "matmul", out=pp.v, lhsT=ymT[bidx][:, kc, off:off + 128], rhs=wo[:, kc, half * 512:(half + 1) * 512],
                      start=(kc == 0), stop=(kc == 7))
            gv = g1c if tt < 2 else g1
            for half, pp in enumerate([pA, pB]):
                I("dve", "tensor_tensor", out=Z[it][:, half * 512:(half + 1) * 512], in0=pp.v,
                  in1=gv[:, half * 512:(half + 1) * 512], op=ALU.mult, _partial=True)
            I("dve", "scalar_tensor_tensor", out=Z[it].v, in0=xt[s].v, scalar=ALPHA, in1=Z[it].v, op0=ALU.mult, op1=ALU.add)
            act_sum(Z[it], junk, s1, it)
        for it in range(ntl):
            act_sum(Z[it], junk, s2, it, sq=True)
        batch_rstd2(s1, s2, mean, rs, ntl)
        P.barrier("wout1")
        A.reset(mZ)
        lg = A.alloc("lg", [128, D], F32)
        lb = A.alloc("lb", [128, D], F32)
        bcast_load(lg.v, lnv[l, 0], D)
        bcast_load(lb.v, lnv[l, 1], D)
        s1b = A.alloc("s1b", [128, NT], F32)
        s2b = A.alloc("s2b", [128, NT], F32)
        mean2 = A.alloc("mean2", [128, NT], F32)
        rs2 = A.alloc("rs2", [128, NT], F32)
        bcast_load(sh1.v, mod_d[l, bi, 3 * D:4 * D], D)
        bcast_load(sc1.v, mod_d[l, bi, 4 * D:5 * D], D)
        bcast_load(sh1c.v, mod_d[l, 2, 3 * D:4 * D], D)
        bcast_load(sc1c.v, mod_d[l, 2, 4 * D:5 * D], D)
        I("dve", "tensor_scalar_add", out=sc1.v, in0=sc1.v, scalar1=1.0)
        I("dve", "tensor_scalar_add", out=sc1c.v, in0=sc1c.v, scalar1=1.0)
        for it, tt in enumerate(tiles):
            I("dve", "scalar_tensor_tensor", out=Z[it].v, in0=Z[it].v, scalar=mean[:, it:it + 1], in1=lg.v,
              op0=ALU.subtract, op1=ALU.mult)
            I("dve", "scalar_tensor_tensor", out=Z[it].v, in0=Z[it].v, scalar=rs[:, it:it + 1], in1=lb.v,
              op0=ALU.mult, op1=ALU.add)
            if it % 4 == 3 or it == ntl - 1:
                i0 = it - (it % 4)
                cnt = it - i0 + 1
                tg = tiles[i0]
                P.dma(x1_d[bi][tg * 128:(tg + cnt) * 128, :].re("(j p) d -> p j d", p=128), A.span(Z[i0:i0 + cnt], cnt),
                      partial=True, extra_reads=Z[i0 + 1:i0 + cnt])
            act_sum(Z[it], junk, s1b, it)
        for it in range(ntl):
            act_sum(Z[it], junk, s2b, it, sq=True)
        batch_rstd2(s1b, s2b, mean2, rs2, ntl)
        hb = [A.alloc("hb%d" % i, [128, D], BF16) for i in range(2)]
        for it, tt in enumerate(tiles):
            s = it % 2
            scv, shv = (sc1c, sh1c) if tt < 2 else (sc1, sh1)
            I("dve", "scalar_tensor_tensor", out=Z[it].v, in0=Z[it].v, scalar=mean2[:, it:it + 1], in1=scv.v,
              op0=ALU.subtract, op1=ALU.mult)
            I("dve", "scalar_tensor_tensor", out=hb[s].v, in0=Z[it].v, scalar=rs2[:, it:it + 1], in1=shv.v,
              op0=ALU.mult, op1=ALU.add)
            transpose_tile(hb[s], hT, tt)
        if debug and l == layers[0] and bi == bis[0]:
            for i, (t0, n) in enumerate(BLKS):
                P.dma(dbg["h2T"][:, :, t0:t0 + n], hT[i].v, partial=True)
        P.barrier()
        A.reset(m0)

    def phase_moe(l, bi):
        last = (l == DEPTH - 1)
        need_ctx = not last
        m0 = A.mark()
        tiles = list(range(2, NT)) + ([0, 1] if need_ctx else [])
        blks = [1, 2, 3, 4] + ([0] if need_ctx else [])
        acc = [A.alloc("acc%d" % tt, [128, D], F32) for tt in range(NT)]
        comb = A.alloc("comb", [128, NT, E], F32)
        m1 = A.mark()
        rws = A.alloc("rws", [128, 8, E], F32)
        rwb = A.alloc("rwb", [128, 8, E], BF16)
        P.dma(rws.v, router_w.v)
        I("dve", "tensor_copy", out=rwb.v, in_=rws.v)
        rb = A.alloc("rb", [128, E], F32)
        bcast_load(rb.v, router_b.v, E)
        ntl = len(tiles)
        it_of = {tt: it for it, tt in enumerate(tiles)}
        W = ntl * E
        Rn = ["sc", "sel", "msk", "selm", "oh1", "oh2"]
        Rb = {k: A.alloc("r_" + k, [128, ntl, E], F32) for k in Rn}
        top1 = A.alloc("r_top1", [128, ntl, 4], F32)
        top2 = A.alloc("r_top2", [128, ntl, 4], F32)
        gs = A.alloc("r_gs", [128, ntl, 4], F32)
        rv = {k: A.alloc("r_" + k, [128, ntl], F32) for k in ["gmax", "m1", "m2", "ws", "rws"]}
        lgp = ps[0]
        for it, tt in enumerate(tiles):
            bidx, off = blk_of(tt)
            for kc in range(8):
                I("pe", "matmul", out=lgp[:, it * E:(it + 1) * E], lhsT=hT[bidx][:, kc, off:off + 128], rhs=rwb[:, kc, :],
                  start=(kc == 0), stop=(kc == 7), _partial=True)
        f3 = lambda b: b.v
        f4 = lambda b: b.v.re("p t (g e) -> p t g e", g=4)
        bc3 = lambda b: b.v.re("p (t o) -> p t o", o=1).bc([128, ntl, E])
        bc4 = lambda b: b.v.re("p t (g o) -> p t g o", o=1).bc([128, ntl, 4, 4])
        I("act", "activation", out=f3(Rb["sc"]), in_=lgp[:, 0:W].re("p (t e) -> p t e", e=E), func=AF.Sigmoid)
        I("dve", "tensor_tensor", out=f3(Rb["sel"]), in0=f3(Rb["sc"]), in1=rb.v.re("p (o e) -> p o e", o=1).bc([128, ntl, E]),
          op=ALU.add)
        I("dve", "tensor_reduce", out=top1.v, in_=f4(Rb["sel"]), axis=AX.X, op=ALU.max)
        I("dve", "tensor_tensor", out=f4(Rb["msk"]), in0=f4(Rb["sel"]), in1=bc4(top1), op=ALU.is_ge)
        I("dve", "scalar_tensor_tensor", out=f3(Rb["msk"]), in0=f3(Rb["msk"]), scalar=-BIG, in1=f3(Rb["sel"]),
          op0=ALU.mult, op1=ALU.add)
        I("dve", "tensor_reduce", out=top2.v, in_=f4(Rb["msk"]), axis=AX.X, op=ALU.max)
        I("dve", "tensor_tensor", out=gs.v, in0=top1.v, in1=top2.v, op=ALU.add)
        I("dve", "tensor_reduce", out=rv["gmax"].v, in_=gs.v, axis=AX.X, op=ALU.max)
        I("dve", "tensor_tensor", out=gs.v, in0=gs.v, in1=rv["gmax"].v.re("p (t o) -> p t o", o=1).bc([128, ntl, 4]), op=ALU.is_ge)
        I("dve", "tensor_scalar", out=gs.v, in0=gs.v, scalar1=-1.0, scalar2=BIG, op0=ALU.add, op1=ALU.mult)
        I("dve", "tensor_tensor", out=f4(Rb["selm"]), in0=f4(Rb["sel"]), in1=bc4(gs), op=ALU.add)
        I("dve", "tensor_reduce", out=rv["m1"].v, in_=f3(Rb["selm"]), axis=AX.X, op=ALU.max)
        I("dve", "tensor_tensor", out=f3(Rb["oh1"]), in0=f3(Rb["selm"]), in1=bc3(rv["m1"]), op=ALU.is_ge)
        I("dve", "scalar_tensor_tensor", out=f3(Rb["selm"]), in0=f3(Rb["oh1"]), scalar=-BIG, in1=f3(Rb["selm"]),
          op0=ALU.mult, op1=ALU.add)
        I("dve", "tensor_reduce", out=rv["m2"].v, in_=f3(Rb["selm"]), axis=AX.X, op=ALU.max)
        I("dve", "tensor_tensor", out=f3(Rb["oh2"]), in0=f3(Rb["selm"]), in1=bc3(rv["m2"]), op=ALU.is_ge)
        I("dve", "tensor_tensor", out=f3(Rb["oh1"]), in0=f3(Rb["oh1"]), in1=f3(Rb["oh2"]), op=ALU.add)
        I("dve", "tensor_tensor", out=f3(Rb["oh1"]), in0=f3(Rb["oh1"]), in1=f3(Rb["sc"]), op=ALU.mult)
        I("dve", "tensor_reduce", out=rv["ws"].v, in_=f3(Rb["oh1"]), axis=AX.X, op=ALU.add)
        I("dve", "reciprocal", out=rv["rws"].v, in_=rv["ws"].v)
        I("dve", "tensor_tensor", out=comb[:, 0:ntl, :], in0=f3(Rb["oh1"]), in1=bc3(rv["rws"]), op=ALU.mult)
        if debug and l == layers[0] and bi == bis[0]:
            P.dma(dbg["comb"].v, comb.v)
        P.barrier()
        A.reset(m1)
        wstg = [A.alloc("wstg%d" % i, [128, 2048], F32) for i in range(3)]
        wgb = [A.alloc("wgb%d" % i, [128, 8, FE], BF16) for i in range(2)]
        wub = [A.alloc("wub%d" % i, [128, 8, FE], BF16) for i in range(2)]
        wdb = [A.alloc("wdb%d" % i, [128, 4, D], BF16) for i in range(2)]
        sgb = [A.alloc("sgm%d" % i, [128, 512], BF16) for i in range(2)]
        heb = [A.alloc("he%d" % i, [128, 4, 512], BF16) for i in range(2)]
        stg_rr = [0]

        def load_expert(e):
            s = e % 2
            for (src, dst) in [(wg, wgb[s]), (wu, wub[s])]:
                for hf in range(2):
                    sg_ = wstg[stg_rr[0] % 3]
                    stg_rr[0] += 1
                    P.dma(sg_.v.re("p (a b) -> p a b", a=4), src[l, e, :, hf * 4:(hf + 1) * 4, :])
                    if e == 0:
                        I("act", "activation", out=dst[:, hf * 4:(hf + 1) * 4, :], in_=sg_.v.re("p (a b) -> p a b", a=4),
                          func=AF.Copy, _partial=True)
                    else:
                        I("pool", "tensor_copy", out=dst[:, hf * 4:(hf + 1) * 4, :], in_=sg_.v.re("p (a b) -> p a b", a=4),
                          _partial=True)
            for hf in range(2):
                sg_ = wstg[stg_rr[0] % 3]
                stg_rr[0] += 1
                P.dma(sg_.v.re("p (a b) -> p a b", a=2), wd[l, e, :, hf * 2:(hf + 1) * 2, :])
                if e == 0:
                    I("dve", "tensor_copy", out=wdb[s][:, hf * 2:(hf + 1) * 2, :], in_=sg_.v.re("p (a b) -> p a b", a=2),
                      _partial=True)
                else:
                    I("pool", "tensor_copy", out=wdb[s][:, hf * 2:(hf + 1) * 2, :], in_=sg_.v.re("p (a b) -> p a b", a=2),
                      _partial=True)

        load_expert(0)
        hcnt = 0

        def gate_up(e, bidx, he):
            s = e % 2
            t0, n = BLKS[bidx]
            for fc in range(4):
                pg = ps[(2 * fc) % 4]
                pu = ps[(2 * fc + 1) % 4]
                for kc in range(8):
                    I("pe", "matmul", out=pg[:, 0:n], lhsT=wgb[s][:, kc, fc * 128:(fc + 1) * 128], rhs=hT[bidx][:, kc, :],
                      start=(kc == 0), stop=(kc == 7))
                for kc in range(8):
                    I("pe", "matmul", out=pu[:, 0:n], lhsT=wub[s][:, kc, fc * 128:(fc + 1) * 128], rhs=hT[bidx][:, kc, :],
                      start=(kc == 0), stop=(kc == 7))
                sg_ = sgb[fc % 2]
                I("act", "activation", out=sg_[:, 0:n], in_=pg[:, 0:n], func=AF.Silu)
                I("dve", "tensor_tensor", out=he[:, fc, 0:n], in0=pu[:, 0:n], in1=sg_[:, 0:n], op=ALU.mult, _partial=True)

        def down(e, bidx, he):
            s = e % 2
            t0, n = BLKS[bidx]
            for qs in range(n // 128):
                tt = t0 // 128 + qs
                for half in range(2):
                    po = ps[4 + half]
                    for fc in range(4):
                        I("pe", "matmul", out=po.v, lhsT=he[:, fc, qs * 128:(qs + 1) * 128],
                          rhs=wdb[s][:, fc, half * 512:(half + 1) * 512], start=(fc == 0), stop=(fc == 3))
                    dst = acc[tt][:, half * 512:(half + 1) * 512]
                    cs = comb[:, it_of[tt], e:e + 1]
                    if e == 0:
                        I("dve", "tensor_scalar", out=dst, in0=po.v, scalar1=cs, scalar2=None,
                          op0=ALU.mult, _partial=True)
                    else:
                        I("dve", "scalar_tensor_tensor", out=dst, in0=po.v, scalar=cs, in1=dst,
                          op0=ALU.mult, op1=ALU.add)

        pending = None
        for e in range(E):
            for bi_, bidx in enumerate(blks):
                he = heb[hcnt % 2]
                hcnt += 1
                gate_up(e, bidx, he)
                if pending is not None:
                    down(*pending)
                pending = (e, bidx, he)
                if bi_ == 0 and e + 1 < E:
                    load_expert(e + 1)
        down(*pending)
        P.barrier()
        A.reset(m1)
        g2 = A.alloc("g2", [128, D], F32)
        g2c = A.alloc("g2c", [128, D], F32)
        lg = A.alloc("lg2", [128, D], F32)
        lb = A.alloc("lb2", [128, D], F32)
        bcast_load(g2.v, mod_d[l, bi, 5 * D:6 * D], D)
        bcast_load(g2c.v, mod_d[l, 2, 5 * D:6 * D], D)
        bcast_load(lg.v, lnv[l, 2], D)
        bcast_load(lb.v, lnv[l, 3], D)
        xg = [A.alloc("mxg%d" % i, [128, 4, D], F32) for i in range(2)]
        s1 = A.alloc("ms1", [128, NT], F32)
        s2 = A.alloc("ms2", [128, NT], F32)
        mean = A.alloc("mmean", [128, NT], F32)
        rs = A.alloc("mrs", [128, NT], F32)
        junk = A.alloc("mjunk", [128, D], BF16)
        ntl = len(tiles)
        fgroups = [(i0, min(4, ntl - i0)) for i0 in range(0, ntl, 4)]
        for gi, (i0, cnt) in enumerate(fgroups):
            tg = tiles[i0]
            xb = xg[gi % 2]
            P.dma(xb[:, 0:cnt, :], x1_d[bi][tg * 128:(tg + cnt) * 128, :].re("(j p) d -> p j d", p=128))
            for it in range(i0, i0 + cnt):
                tt = tiles[it]
                if debug and l == layers[0] and bi == bis[0]:
                    P.dma(dbg["acc"][tt * 128:(tt + 1) * 128, :], acc[tt].v, partial=True)
                gv = g2c if tt < 2 else g2
                I("dve", "tensor_tensor", out=acc[tt].v, in0=acc[tt].v, in1=gv.v, op=ALU.mult)
                I("dve", "scalar_tensor_tensor", out=acc[tt].v, in0=xb[:, it - i0, :], scalar=ALPHA, in1=acc[tt].v,
                  op0=ALU.mult, op1=ALU.add)
                act_sum(acc[tt], junk, s1, it)
        for it, tt in enumerate(tiles):
            act_sum(acc[tt], junk, s2, it, sq=True)
        batch_rstd2(s1, s2, mean, rs, ntl)
        for gi, (i0, cnt) in enumerate(fgroups):
            tg = tiles[i0]
            for it in range(i0, i0 + cnt):
                tt = tiles[it]
                I("dve", "scalar_tensor_tensor", out=acc[tt].v, in0=acc[tt].v, scalar=mean[:, it:it + 1], in1=lg.v,
                  op0=ALU.subtract, op1=ALU.mult)
                I("dve", "scalar_tensor_tensor", out=acc[tt].v, in0=acc[tt].v, scalar=rs[:, it:it + 1], in1=lb.v,
                  op0=ALU.mult, op1=ALU.add)
            src = A.span(acc[tg:tg + cnt], cnt)
            if last:
                dst = out_d[bi, (tg - 2) * 128:(tg - 2 + cnt) * 128, :]
            else:
                dst = xres_d[bi][tg * 128:(tg + cnt) * 128, :]
            P.dma(dst.re("(j p) d -> p j d", p=128), src, partial=True, extra_reads=acc[tg + 1:tg + cnt])
        P.barrier()
        A.reset(m0)

    with nc.allow_low_precision("bf16 matmul operands, fp32 accumulation"):
        with nc.Block() as block:
            phase_mod()
            for bi in bis:
                for l in layers:
                    phase_mix(l, bi)
                    if stop_after in ("lnt", "mix"):
                        continue
                    phase_moe(l, bi)
            P.barrier()

            @block.sync
            def _(e):
                P.replay("sp", e)

            @block.scalar
            def _(e):
                P.replay("act", e)

            @block.vector
            def _(e):
                P.replay("dve", e)

            @block.gpsimd
            def _(e):
                P.replay("pool", e)

            @block.tensor
            def _(e):
                P.replay("pe", e)
    nc._prog_stats = (P.n_ins, {k: len(v.ops) for k, v in P.E.items()})
    nc._marks = P.marks
    return nc


def _pack_kp(w, p=128):
    sh = w.shape
    k, n = sh[-2], sh[-1]
    w = w.reshape(sh[:-2] + (k // p, p, n))
    nd = w.ndim
    perm = list(range(nd - 3)) + [nd - 2, nd - 3, nd - 1]
    return np.ascontiguousarray(w.transpose(perm))


def prepare_inputs(x, c, ctx, c_ctx, w_mod, b_mod, w_in, w_out, conv_w, conv_b, conv_norm_g, conv_norm_b,
                   diff_lambda, diff_subln_g, win_sink, ln_mix_g, ln_mix_b, ln_ffn_g, ln_ffn_b,
                   router_w, router_bias, exp_w_gate, exp_w_up, exp_w_down):
    f = lambda a: np.ascontiguousarray(np.asarray(a, dtype=np.float32))
    x, c, ctx, c_ctx = f(x), f(c), f(ctx), f(c_ctx)
    perm = _perm_w_in()
    w_in = f(w_in)
    wx = w_in[:, :, perm]
    wx = wx.reshape(DEPTH, 8, 128, NCH, 128).transpose(0, 3, 2, 1, 4)
    shared = {
        "w_mod": _pack_kp(f(w_mod)),
        "b_mod": f(b_mod),
        "w_inx": np.ascontiguousarray(wx),
        "w_out": _pack_kp(f(w_out)),
        "conv_w": np.ascontiguousarray(f(conv_w).reshape(DEPTH, 31, 2, 128).transpose(0, 2, 3, 1)),
        "conv_v": np.ascontiguousarray(np.stack([f(conv_b), f(conv_norm_g), f(conv_norm_b)], -1).reshape(DEPTH, 2, 128, 3)),
        "dlam": f(diff_lambda).reshape(DEPTH, 128),
        "dsub": f(diff_subln_g),
        "wsink": f(win_sink),
        "lnv": np.ascontiguousarray(np.stack([f(ln_mix_g), f(ln_mix_b), f(ln_ffn_g), f(ln_ffn_b)], 1)),
        "router_w": _pack_kp(f(router_w)),
        "router_b": f(router_bias),
        "wg": _pack_kp(f(exp_w_gate)),
        "wu": _pack_kp(f(exp_w_up)),
        "wd": _pack_kp(f(exp_w_down)),
    }
    cst = _consts()
    shared["c_rope"] = cst["rope"]
    shared["c_f64"] = cst["f64"]
    shared["c_fN"] = np.ascontiguousarray(cst["fN"])
    shared["c_fC"] = np.ascontiguousarray(cst["fC"])
    shared["c_wmask"] = cst["wmask"]
    shared["c_ident"] = cst["ident"]
    shared["c_bmask"] = cst["bmask"]
    in_maps = []
    for core in range(NCORE):
        b0 = core * BPC
        m = dict(shared)
        m["x"] = x[b0:b0 + BPC]
        m["ctx"] = ctx[b0:b0 + BPC]
        cs = np.stack([c[b0], c[b0 + 1], c_ctx], -1)
        m["cT"] = np.ascontiguousarray(cs.reshape(8, 128, 3).transpose(1, 0, 2))
        in_maps.append(m)
    return in_maps


_NC_CACHE = {}


def kernel(**inputs):
    in_maps = prepare_inputs(**inputs)
    if "nc" not in _NC_CACHE:
        _NC_CACHE["nc"] = build_nc()
    nc = _NC_CACHE["nc"]
    res = run_bass_kernel_spmd(nc, in_maps, core_ids=list(range(NCORE)))
    out = np.concatenate([np.asarray(r["out"]) for r in res.results], axis=0)
    return out.astype(np.float32)
```
